# Optimizing a Trainium2 kernel written in Bass

```python
import math
import jax
import jax.numpy as jnp
from jax import lax
import numpy as np

D_MODEL = 2048
BATCH = 2
SEQ = 4096
DEPTH = 2
DEC_BATCH = 8
DEC_SEQ = 1
PAST_LEN = 16384
PAGE_SIZE = 128

MEM_LEN = 256
N_MEM_HEADS = 4
MEM_HEAD_DIM = 128
MIX_W = D_MODEL
MEM_W = N_MEM_HEADS * MEM_HEAD_DIM
TOK_W = MIX_W - MEM_W
D_FF = 5632
NORM_EPS = 1e-6
GLA_HEADS = 4
GLA_DV = TOK_W // GLA_HEADS
GLA_DK = GLA_DV // 2
GLA_GATE_RANK = 16
GLA_TAU = 16.0
GLA_CHUNK = 64
NSA_HEAD_DIM = 128
NSA_HEADS = TOK_W // NSA_HEAD_DIM
NSA_GROUPS = 3
NSA_REP = NSA_HEADS // NSA_GROUPS
NSA_BLOCK = 64
NSA_TOPN = 16
NSA_WINDOW = 512
NSA_CMP_HIDDEN = 256
NSA_QBLK = 64
NSA_KV_W = NSA_GROUPS * NSA_HEAD_DIM
REL_BUCKETS = 32
REL_MAX_EXACT = 16
REL_MAX_DIST = 128
N_GLA = (DEPTH + 1) // 2
N_NSA = DEPTH // 2
GLA_COLS = 2 * GLA_HEADS * GLA_DK + 2 * TOK_W + GLA_GATE_RANK + MEM_W
NSA_COLS = TOK_W + 6 * NSA_KV_W + 3 * NSA_HEADS + MEM_W

kernel_name = 'hybrid_gla_nsa_decoder_step'


def rmsnorm(x, g):
    xf = x.astype(jnp.float32)
    y = xf * lax.rsqrt(jnp.mean(xf * xf, axis=-1, keepdims=True) + NORM_EPS)
    return (y * g.astype(jnp.float32)).astype(x.dtype)


def swiglu(x, w_gate, w_up, w_down):
    return (jax.nn.silu(x @ w_gate) * (x @ w_up)) @ w_down


def masked_softmax(s, valid):
    s = jnp.where(valid, s.astype(jnp.float32), -jnp.inf)
    m = jnp.max(s, axis=-1, keepdims=True)
    m = jnp.where(jnp.isfinite(m), m, 0.0)
    e = jnp.exp(s - m)
    return e / jnp.maximum(jnp.sum(e, axis=-1, keepdims=True), 1e-30)


def t5_bucket(dist):
    n = jnp.maximum(dist, 0)
    nf = jnp.maximum(n, REL_MAX_EXACT).astype(jnp.float32)
    large = REL_MAX_EXACT + (jnp.log(nf / REL_MAX_EXACT) / math.log(REL_MAX_DIST / REL_MAX_EXACT)
                             * (REL_BUCKETS - REL_MAX_EXACT)).astype(jnp.int32)
    return jnp.where(n < REL_MAX_EXACT, n, jnp.minimum(large, REL_BUCKETS - 1))


def mem_attend(qm, mem_kv):
    s = jnp.einsum('blhd,bmhd->bhlm', qm, mem_kv[:, :, 0]) * (MEM_HEAD_DIM ** -0.5)
    p = jax.nn.softmax(s.astype(jnp.float32), axis=-1).astype(mem_kv.dtype)
    o = jnp.einsum('bhlm,bmhd->blhd', p, mem_kv[:, :, 1])
    return o.reshape(qm.shape[0], qm.shape[1], MEM_W)


def gla_recurrence(q, k, v, log_a, s0):
    B, L, H = q.shape[:3]
    c = min(GLA_CHUNK, L)
    n = -(-L // c)
    pad = n * c - L

    def prep(a):
        a = jnp.pad(a.astype(jnp.float32), ((0, 0), (0, pad), (0, 0), (0, 0)))
        return jnp.moveaxis(a.reshape(B, n, c, a.shape[2], a.shape[3]), 1, 0)

    causal = jnp.tril(jnp.ones((c, c), dtype=bool))

    def step(S, xs):
        qc, kc, vc, ac = xs
        b = jnp.cumsum(ac, axis=1)
        qe = qc * jnp.exp(b)
        ke = kc * jnp.exp(-b)
        o = jnp.einsum('bchk,bhkv->bchv', qe, S)
        A = jnp.where(causal, jnp.einsum('bchk,bshk->bhcs', qe, ke), 0.0)
        o = o + jnp.einsum('bhcs,bshv->bchv', A, vc)
        bl = b[:, -1]
        S = jnp.exp(bl)[..., None] * S + jnp.einsum('bshk,bshv->bhkv', kc * jnp.exp(bl[:, None] - b), vc)
        return S, o

    S, o = lax.scan(step, s0.astype(jnp.float32), (prep(q), prep(k), prep(v), prep(log_a)))
    o = jnp.moveaxis(o, 0, 1).reshape(B, n * c, H, v.shape[3])[:, :L]
    return o, S


def gla_mixer(tp, s0, w_a2, b_a, onorm_g):
    B, L, _ = tp.shape
    dkw = GLA_HEADS * GLA_DK
    q = tp[..., :dkw].reshape(B, L, GLA_HEADS, GLA_DK) * (GLA_DK ** -0.5)
    k = tp[..., dkw:2 * dkw].reshape(B, L, GLA_HEADS, GLA_DK)
    v = tp[..., 2 * dkw:2 * dkw + TOK_W].reshape(B, L, GLA_HEADS, GLA_DV)
    r = tp[..., 2 * dkw + TOK_W:2 * dkw + 2 * TOK_W]
    a = tp[..., 2 * dkw + 2 * TOK_W:]
    log_a = (jax.nn.log_sigmoid((a @ w_a2 + b_a).astype(jnp.float32)) / GLA_TAU).reshape(B, L, GLA_HEADS, GLA_DK)
    o, s_new = gla_recurrence(q, k, v, log_a, s0)
    o = rmsnorm(o.astype(tp.dtype), onorm_g).reshape(B, L, TOK_W)
    return o * jax.nn.silu(r), s_new.astype(tp.dtype)


def compress_blocks(rows, pe, w1, b1, w2):
    B, Tp, G, dh = rows.shape
    nb = Tp // NSA_BLOCK
    blk = rows.reshape(B, nb, NSA_BLOCK, G, dh) + pe[:, None, :]
    blk = jnp.swapaxes(blk, 2, 3).reshape(B, nb, G, NSA_BLOCK * dh)
    return jax.nn.silu(blk @ w1 + b1) @ w2


def nsa_core(q, gates, cmp_all, slc_all, win_all, off, rel_bias, cmp_pe, cmp_w1, cmp_b1, cmp_w2):
    B, Lq = q.shape[:2]
    G, R, dh = NSA_GROUPS, NSA_REP, NSA_HEAD_DIM
    T = off + Lq
    nb = -(-T // NSA_BLOCK)
    Tp = nb * NSA_BLOCK

    def pad_t(a):
        return jnp.pad(a, ((0, 0), (0, Tp - T), (0, 0), (0, 0)))

    k_cmp = compress_blocks(pad_t(cmp_all[:, :, 0]), cmp_pe[0], cmp_w1[0], cmp_b1[0], cmp_w2[0])
    v_cmp = compress_blocks(pad_t(cmp_all[:, :, 1]), cmp_pe[1], cmp_w1[1], cmp_b1[1], cmp_w2[1])
    k_slc = jnp.swapaxes(pad_t(slc_all[:, :, 0]), 1, 2)
    v_slc = jnp.swapaxes(pad_t(slc_all[:, :, 1]), 1, 2)
    k_win, v_win = win_all[:, :, 0], win_all[:, :, 1]
    blk_idx = jnp.arange(nb)
    blk_end = (blk_idx + 1) * NSA_BLOCK - 1
    n_sel = min(NSA_TOPN, nb)
    n_keys_sel = n_sel * NSA_BLOCK
    qb = math.gcd(Lq, NSA_QBLK)
    nq = Lq // qb
    n_keys_win = NSA_WINDOW + qb
    bias_grp = jnp.swapaxes(rel_bias.reshape(REL_BUCKETS, G, R), 0, 1)
    q_blocks = jnp.moveaxis((q * (dh ** -0.5)).reshape(B, nq, qb, G, R, dh), 1, 0)
    g_blocks = jnp.moveaxis(gates.reshape(B, nq, qb, G, R, 3), 1, 0)
    starts = jnp.arange(nq, dtype=jnp.int32) * qb
    b_ix = jnp.arange(B)[:, None, None]
    g_ix = jnp.arange(G)[None, :, None]

    def one_block(args):
        qg, gg, qs = args
        t = off + qs + jnp.arange(qb)
        dist_c = t[:, None] - blk_end[None, :]
        bias_c = jnp.transpose(rel_bias[t5_bucket(dist_c)].reshape(qb, nb, G, R), (0, 2, 3, 1))
        s_c = jnp.einsum('bqgrd,bngd->bqgrn', qg, k_cmp) + bias_c
        p_c = masked_softmax(s_c, (dist_c >= 0)[:, None, None, :])
        o_c = jnp.einsum('bqgrn,bngd->bqgrd', p_c.astype(v_cmp.dtype), v_cmp)
        imp = jnp.sum(p_c, axis=3)
        tb = (t // NSA_BLOCK)[:, None]
        forced = (blk_idx[None, :] == 0) | (blk_idx[None, :] == tb) | (blk_idx[None, :] == tb - 1)
        future = blk_idx[None, :] * NSA_BLOCK > t[:, None]
        score = jnp.where(forced[:, None, :], jnp.inf, jnp.where(future[:, None, :], -jnp.inf, imp))
        _, sel = lax.top_k(score, n_sel)
        tok = (sel[..., None] * NSA_BLOCK + jnp.arange(NSA_BLOCK)).reshape(B, qb, G, n_keys_sel)
        tok_bg = jnp.swapaxes(tok, 1, 2).reshape(B, G, qb * n_keys_sel)
        ks_g = k_slc[b_ix, g_ix, tok_bg].reshape(B, G, qb, n_keys_sel, dh)
        vs_g = v_slc[b_ix, g_ix, tok_bg].reshape(B, G, qb, n_keys_sel, dh)
        dist_s = t[None, :, None, None] - tok
        bias_s = jnp.moveaxis(bias_grp[jnp.arange(G)[None, None, :, None], t5_bucket(dist_s)], -1, 3)
        s_s = jnp.einsum('bqgrd,bgqld->bqgrl', qg, ks_g) + bias_s
        p_s = masked_softmax(s_s, (dist_s >= 0)[:, :, :, None, :])
        o_s = jnp.einsum('bqgrl,bgqld->bqgrd', p_s.astype(vs_g.dtype), vs_g)
        kw = lax.dynamic_slice_in_dim(k_win, qs, n_keys_win, axis=1)
        vw = lax.dynamic_slice_in_dim(v_win, qs, n_keys_win, axis=1)
        s_pos = off - NSA_WINDOW + qs + jnp.arange(n_keys_win)
        dist_w = t[:, None] - s_pos[None, :]
        valid_w = (dist_w >= 0) & (dist_w <= NSA_WINDOW) & (s_pos[None, :] >= 0)
        bias_w = jnp.transpose(rel_bias[t5_bucket(dist_w)].reshape(qb, n_keys_win, G, R), (0, 2, 3, 1))
        s_w = jnp.einsum('bqgrd,bkgd->bqgrk', qg, kw) + bias_w
        p_w = masked_softmax(s_w, valid_w[:, None, None, :])
        o_w = jnp.einsum('bqgrk,bkgd->bqgrd', p_w.astype(vw.dtype), vw)
        o = gg[..., 0:1] * o_c + gg[..., 1:2] * o_s + gg[..., 2:3] * o_w
        return o.reshape(B, qb, TOK_W)

    out = lax.map(one_block, (q_blocks, g_blocks, starts))
    return jnp.moveaxis(out, 0, 1).reshape(B, Lq, TOK_W)


def nsa_mixer(tp, past, off, win_buf, rel_bias, gate_b, cmp_pe, cmp_w1, cmp_b1, cmp_w2):
    B, L, _ = tp.shape
    q = tp[..., :TOK_W].reshape(B, L, NSA_HEADS, NSA_HEAD_DIM)
    kv = tp[..., TOK_W:TOK_W + 6 * NSA_KV_W].reshape(B, L, 6, NSA_GROUPS, NSA_HEAD_DIM)
    gates = jax.nn.sigmoid(tp[..., TOK_W + 6 * NSA_KV_W:] + gate_b).reshape(B, L, NSA_HEADS, 3)
    new_cmp, new_slc, new_win = kv[:, :, 0:2], kv[:, :, 2:4], kv[:, :, 4:6]
    if past is None:
        cmp_all, slc_all = new_cmp, new_slc
        win_all = jnp.pad(new_win, ((0, 0), (NSA_WINDOW, 0), (0, 0), (0, 0), (0, 0)))
    else:
        cmp_past, slc_past, win_past = past
        cmp_all = jnp.concatenate([cmp_past, new_cmp], axis=1)
        slc_all = jnp.concatenate([slc_past, new_slc], axis=1)
        win_past = jnp.pad(win_past, ((0, 0), (NSA_WINDOW - win_past.shape[1], 0), (0, 0), (0, 0), (0, 0)))
        win_all = jnp.concatenate([win_past, new_win], axis=1)
    out = nsa_core(q, gates, cmp_all, slc_all, win_all, off, rel_bias, cmp_pe, cmp_w1, cmp_b1, cmp_w2)
    return out, (new_cmp, new_slc, win_all[:, -win_buf:])


def setup_inputs(seed: int = 0) -> dict:
    key = jax.random.key(seed)
    ks = jax.random.split(key, 32)

    def nrm(k, shape, scale=1.0):
        return jax.random.normal(k, shape, jnp.float32) * scale

    n_pages = PAST_LEN // PAGE_SIZE
    n_used = DEC_BATCH * n_pages
    n_pool = (5 * n_used + 3) // 4
    win_buf = min(NSA_WINDOW, PAST_LEN)
    page_table = jax.random.permutation(ks[0], n_pool)[:n_used].reshape(DEC_BATCH, n_pages).astype(jnp.int32)
    kv_shape = (N_NSA, n_pool, PAGE_SIZE, 2, NSA_GROUPS, NSA_HEAD_DIM)
    return {
        'x_prompt': nrm(ks[1], (BATCH, SEQ, D_MODEL)),
        'x_sample': nrm(ks[2], (DEC_BATCH, DEC_SEQ, D_MODEL)),
        'mem_prompt': nrm(ks[3], (BATCH, MEM_LEN, D_MODEL)),
        'cache_cmp_kv': nrm(ks[4], kv_shape),
        'cache_slc_kv': nrm(ks[5], kv_shape),
        'state_win_kv': nrm(ks[6], (N_NSA, DEC_BATCH, win_buf, 2, NSA_GROUPS, NSA_HEAD_DIM)),
        'state_gla': nrm(ks[7], (N_GLA, DEC_BATCH, GLA_HEADS, GLA_DK, GLA_DV)),
        'cache_mem_kv': nrm(ks[8], (DEPTH, DEC_BATCH, MEM_LEN, 2, N_MEM_HEADS, MEM_HEAD_DIM)),
        'page_table': page_table,
        'norm_g': 1.0 + nrm(ks[9], (DEPTH, 6, D_MODEL), 0.02),
        'w_ffn_gate': nrm(ks[10], (DEPTH, 2, D_MODEL, D_FF), D_MODEL ** -0.5),
        'w_ffn_up': nrm(ks[11], (DEPTH, 2, D_MODEL, D_FF), D_MODEL ** -0.5),
        'w_ffn_down': nrm(ks[12], (DEPTH, 2, D_FF, D_MODEL), D_FF ** -0.5),
        'w_in_gla': nrm(ks[13], (N_GLA, D_MODEL, GLA_COLS), D_MODEL ** -0.5),
        'w_in_nsa': nrm(ks[14], (N_NSA, D_MODEL, NSA_COLS), D_MODEL ** -0.5),
        'w_out': nrm(ks[15], (DEPTH, MIX_W, D_MODEL), MIX_W ** -0.5),
        'mem_norm_g': 1.0 + nrm(ks[16], (DEPTH, D_MODEL), 0.02),
        'w_mem_kv': nrm(ks[17], (DEPTH, D_MODEL, 2 * MEM_W), D_MODEL ** -0.5),
        'w_gla_a2': nrm(ks[18], (N_GLA, GLA_GATE_RANK, GLA_HEADS * GLA_DK), GLA_GATE_RANK ** -0.5),
        'b_gla_a': nrm(ks[19], (N_GLA, GLA_HEADS * GLA_DK), 0.1),
        'gla_onorm_g': 1.0 + nrm(ks[20], (N_GLA, GLA_DV), 0.02),
        'nsa_gate_b': nrm(ks[21], (N_NSA, 3 * NSA_HEADS), 0.1),
        'cmp_pe': nrm(ks[22], (N_NSA, 2, NSA_BLOCK, NSA_HEAD_DIM), 0.02),
        'cmp_w1': nrm(ks[23], (N_NSA, 2, NSA_BLOCK * NSA_HEAD_DIM, NSA_CMP_HIDDEN), (NSA_BLOCK * NSA_HEAD_DIM) ** -0.5),
        'cmp_b1': nrm(ks[24], (N_NSA, 2, NSA_CMP_HIDDEN), 0.02),
        'cmp_w2': nrm(ks[25], (N_NSA, 2, NSA_CMP_HIDDEN, NSA_HEAD_DIM), NSA_CMP_HIDDEN ** -0.5),
        'rel_bias': nrm(ks[26], (REL_BUCKETS, NSA_HEADS), 0.2),
    }


def reference(x_prompt, x_sample, mem_prompt, cache_cmp_kv, cache_slc_kv, state_win_kv, state_gla, cache_mem_kv,
              page_table, norm_g, w_ffn_gate, w_ffn_up, w_ffn_down, w_in_gla, w_in_nsa, w_out, mem_norm_g, w_mem_kv,
              w_gla_a2, b_gla_a, gla_onorm_g, nsa_gate_b, cmp_pe, cmp_w1, cmp_b1, cmp_w2, rel_bias):
    past_len = page_table.shape[1] * cache_cmp_kv.shape[2]
    win_buf = state_win_kv.shape[2]
    n_dec = x_sample.shape[0]

    def ffn_half(x, i, j):
        h = rmsnorm(x, norm_g[i, 4 * j])
        y = swiglu(h, w_ffn_gate[i, j], w_ffn_up[i, j], w_ffn_down[i, j])
        return x + 0.5 * rmsnorm(y, norm_g[i, 4 * j + 1])

    def mixing(x, i, mem_kv, gla_s0, nsa_past, off):
        B, L, _ = x.shape
        li = i // 2
        h = rmsnorm(x, norm_g[i, 2])
        if i % 2 == 0:
            proj = h @ w_in_gla[li]
            tok, s_new = gla_mixer(proj[..., :GLA_COLS - MEM_W], gla_s0, w_gla_a2[li], b_gla_a[li], gla_onorm_g[li])
            new = (s_new,)
        else:
            proj = h @ w_in_nsa[li]
            tok, new = nsa_mixer(proj[..., :NSA_COLS - MEM_W], nsa_past, off, win_buf, rel_bias, nsa_gate_b[li],
                                 cmp_pe[li], cmp_w1[li], cmp_b1[li], cmp_w2[li])
        mem_o = mem_attend(proj[..., -MEM_W:].reshape(B, L, N_MEM_HEADS, MEM_HEAD_DIM), mem_kv)
        y = jnp.concatenate([tok, mem_o], axis=-1) @ w_out[i]
        return x + rmsnorm(y, norm_g[i, 3]), new

    def to_pages(a):
        return a.reshape(a.shape[0], a.shape[1] // PAGE_SIZE, PAGE_SIZE, *a.shape[2:])

    xp, xs = x_prompt, x_sample
    gla_p, gla_s, cmp_p, cmp_s, slc_p, slc_s, win_p, win_s, mem_p = [], [], [], [], [], [], [], [], []
    for i in range(DEPTH):
        li = i // 2
        mem_kv_p = (rmsnorm(mem_prompt, mem_norm_g[i]) @ w_mem_kv[i]).reshape(
            mem_prompt.shape[0], mem_prompt.shape[1], 2, N_MEM_HEADS, MEM_HEAD_DIM)
        mem_p.append(mem_kv_p)
        xp = ffn_half(xp, i, 0)
        xs = ffn_half(xs, i, 0)
        if i % 2 == 0:
            s0 = jnp.zeros((xp.shape[0], GLA_HEADS, GLA_DK, GLA_DV), xp.dtype)
            xp, (sp,) = mixing(xp, i, mem_kv_p, s0, None, 0)
            xs, (ss,) = mixing(xs, i, cache_mem_kv[i], state_gla[li], None, past_len)
            gla_p.append(sp)
            gla_s.append(ss)
        else:
            past = (cache_cmp_kv[li][page_table].reshape(n_dec, past_len, 2, NSA_GROUPS, NSA_HEAD_DIM),
                    cache_slc_kv[li][page_table].reshape(n_dec, past_len, 2, NSA_GROUPS, NSA_HEAD_DIM),
                    state_win_kv[li])
            xp, (cp, sp2, wp) = mixing(xp, i, mem_kv_p, None, None, 0)
            xs, (cs, ss2, ws) = mixing(xs, i, cache_mem_kv[i], None, past, past_len)
            cmp_p.append(to_pages(cp))
            slc_p.append(to_pages(sp2))
            win_p.append(wp)
            cmp_s.append(cs)
            slc_s.append(ss2)
            win_s.append(ws)
        xp = ffn_half(xp, i, 1)
        xs = ffn_half(xs, i, 1)
    y_prompt, y_sample = xp, xs
    return (y_prompt, y_sample, jnp.stack(gla_p), jnp.stack(cmp_p), jnp.stack(slc_p), jnp.stack(win_p),
            jnp.stack(mem_p), jnp.stack(gla_s), jnp.stack(cmp_s), jnp.stack(slc_s), jnp.stack(win_s))
```

```python
import numpy as np
from contextlib import ExitStack
import concourse.bass as bass
import concourse.mybir as mybir
from concourse.bass_utils import run_bass_kernel_spmd

F32 = mybir.dt.float32
BF16 = mybir.dt.bfloat16
I32 = mybir.dt.int32
AF = mybir.ActivationFunctionType
ALU = mybir.AluOpType
AX = mybir.AxisListType

D = 2048
KC = 16
DFF = 5632
FT = 44
EPS = 1e-6
NPRE = 1
SEG = 1024


class Buf:
    __slots__ = ("name", "w", "rs")

    def __init__(self, name=""):
        self.name = name
        self.w = None
        self.rs = []


class Prog:
    ENGS = ("pe", "act", "dve", "pool", "sp")

    def __init__(self, nc, n_dma_sems=8):
        self.nc = nc
        self.streams = {e: [] for e in self.ENGS}
        self.cnt = {e: 0 for e in self.ENGS}
        self.sem = {}
        self.seen = {e: {} for e in self.ENGS}
        self.n_dma_sems = n_dma_sems
        self.dma_rr = {q: 0 for q in ("sp", "act", "pool")}
        self.dma_cnt = {}

    def alloc_sems(self, stack):
        nc = self.nc
        for e in self.ENGS:
            self.sem["c_" + e] = stack.enter_context(nc.semaphore("c_" + e))
        for q in ("sp", "act", "pool"):
            for i in range(self.n_dma_sems):
                nm = "d_%s_%d" % (q, i)
                self.sem[nm] = stack.enter_context(nc.semaphore(nm))
                self.dma_cnt[nm] = 0

    def _deps(self, eng, reads, writes):
        toks = []
        for b in reads:
            if b.w is not None:
                toks.append(b.w)
        for b in writes:
            if b.w is not None:
                toks.append(b.w)
            toks.extend(b.rs)
        need = {}
        for (nm, val) in toks:
            if self.seen[eng].get(nm, 0) >= val:
                continue
            if need.get(nm, 0) < val:
                need[nm] = val
        for nm, val in need.items():
            self.seen[eng][nm] = val
        return list(need.items())

    def _commit(self, tok, reads, writes):
        for b in writes:
            b.w = tok
            b.rs = []
        for b in reads:
            if b not in writes:
                b.rs.append(tok)
                if len(b.rs) > 16:
                    best = {}
                    for (nm, v) in b.rs:
                        if best.get(nm, 0) < v:
                            best[nm] = v
                    b.rs = list(best.items())

    def op(self, eng, fns, reads=(), writes=()):
        if callable(fns):
            fns = [fns]
        reads = list(reads)
        writes = list(writes)
        waits = self._deps(eng, reads, writes)
        self.cnt[eng] += 1
        nm = "c_" + eng
        tok = (nm, self.cnt[eng])
        sem = self.sem

        def emit(e, waits=waits, fns=fns, nm=nm):
            for (wn, wv) in waits:
                e.wait_ge(sem[wn], wv)
            for f in fns[:-1]:
                f(e)
            fns[-1](e).then_inc(sem[nm], 1)
        self.streams[eng].append(emit)
        self._commit(tok, reads, writes)
        return tok

    def dma(self, q, fn, reads=(), writes=()):
        reads = list(reads)
        writes = list(writes)
        i = self.dma_rr[q]
        self.dma_rr[q] = (i + 1) % self.n_dma_sems
        nm = "d_%s_%d" % (q, i)
        waits = self._deps(q, reads, writes)
        self.dma_cnt[nm] += 16
        tok = (nm, self.dma_cnt[nm])
        sem = self.sem

        def emit(e, waits=waits, fn=fn, nm=nm):
            for (wn, wv) in waits:
                e.wait_ge(sem[wn], wv)
            fn(e).then_inc(sem[nm], 16)
        self.streams[q].append(emit)
        self._commit(tok, reads, writes)
        return tok

    def fence(self):
        allw = [("c_" + e, self.cnt[e]) for e in self.ENGS if self.cnt[e] > 0]
        allw += [(nm, v) for nm, v in self.dma_cnt.items() if v > 0]
        sem = self.sem
        for eng in self.ENGS:
            waits = [(nm, v) for (nm, v) in allw if self.seen[eng].get(nm, 0) < v]
            for nm, v in waits:
                self.seen[eng][nm] = v

            def emit(e, waits=waits):
                for (wn, wv) in waits:
                    e.wait_ge(sem[wn], wv)
            self.streams[eng].append(emit)

    def emit_all(self, block):
        st = self.streams

        @block.tensor
        def _(e):
            for f in st["pe"]:
                f(e)

        @block.scalar
        def _(e):
            for f in st["act"]:
                f(e)

        @block.vector
        def _(e):
            for f in st["dve"]:
                f(e)

        @block.gpsimd
        def _(e):
            for f in st["pool"]:
                f(e)

        @block.sync
        def _(e):
            for f in st["sp"]:
                f(e)


class Builder:
    def __init__(self, stages):
        self.stages = stages
        self.nc = bass.Bass("TRN2", target_bir_lowering=False)
        self.P = Prog(self.nc)
        self.inputs = {}
        self.outputs = {}
        self.rr = {}

    def din(self, name, shape, dt=F32):
        t = self.nc.dram_tensor(name, list(shape), dt, kind="ExternalInput").ap()
        self.inputs[name] = t
        return t

    def dout(self, name, shape, dt=F32):
        t = self.nc.dram_tensor(name, list(shape), dt, kind="ExternalOutput").ap()
        self.outputs[name] = t
        return t

    def sb(self, st, name, shape, dt=F32):
        self.uid = getattr(self, "uid", 0) + 1
        return st.enter_context(self.nc.sbuf_tensor("%s_%d" % (name, self.uid), list(shape), dt))

    def norm_stats(self, xs, xb, subt, scale_extra=1.0):
        P = self.P
        for si, (c0, w) in enumerate(subt):
            bank = self.ps[4 + si]
            bbank = self.bps[4 + si]
            for c in range(KC):
                q = self.sq[c % 2]
                bq = self.bsq[c % 2]
                P.op("act", lambda e, q=q, c=c, c0=c0, w=w: e.activation(out=q[:, 0:w], in_=xs[:, c, c0:c0 + w], func=AF.Square),
                     reads=[xb], writes=[bq])
                P.op("pe", lambda e, q=q, c=c, bank=bank, w=w: e.matmul(bank[:, 0:w], self.ones[:, :], q[:, 0:w],
                                                                    start=(c == 0), stop=(c == KC - 1)),
                     reads=[bq, self.bconst], writes=[bbank])
            P.op("act", lambda e, bank=bank, c0=c0, w=w: e.activation(out=self.rstd[:, c0:c0 + w], in_=bank[:, 0:w], func=AF.Ln,
                                                                      scale=1.0 / D, bias=EPS),
                 reads=[bbank], writes=[self.brstd])
            P.op("act", lambda e, c0=c0, w=w: e.activation(out=self.rstd[:, c0:c0 + w], in_=self.rstd[:, c0:c0 + w], func=AF.Exp,
                                                          scale=-0.5, bias=float(np.log(scale_extra))),
                 reads=[self.brstd], writes=[self.brstd])

    def norm_apply(self, xs, xb, hs, hb, subt, gidx):
        P = self.P
        W = sum(w for _, w in subt)
        c00 = subt[0][0]
        for c in range(KC):
            P.op("dve", lambda e, c=c: e.scalar_tensor_tensor(out=hs[:, c, c00:c00 + W], in0=xs[:, c, c00:c00 + W],
                                                              scalar=self.gsb[:, gidx * KC + c:gidx * KC + c + 1],
                                                              in1=self.rstd[:, c00:c00 + W],
                                                              op0=ALU.mult, op1=ALU.mult),
                 reads=[xb, self.brstd, self.bconst], writes=[hb])

    def ffn_pass(self, xs, xb, subt, layer, half, wg, wu, wd):
        P = self.P
        nc = self.nc
        W = sum(w for _, w in subt)
        c00 = subt[0][0]
        gi = layer * 6 + 4 * half
        with ExitStack() as st:
            hs = self.sb(st, "hsf", [128, KC, 513], BF16)
            hb = Buf("h")
            inter = self.sb(st, "inter", [128, FT, W], BF16)
            ys = self.sb(st, "ys", [128, KC, W], F32)
            NB = 2
            wgs = [self.sb(st, "wg%d" % i, [128, KC, 128], BF16) for i in range(NB)]
            wus = [self.sb(st, "wu%d" % i, [128, KC, 128], BF16) for i in range(NB)]
            wds = [self.sb(st, "wd%d" % i, [128, FT, 128], BF16) for i in range(2)]
            sil = [self.sb(st, "sil%d" % i, [128, W], F32) for i in range(2)]
            bwg = [Buf() for _ in range(NB)]
            bwu = [Buf() for _ in range(NB)]
            bwd = [Buf() for _ in range(2)]
            bsil = [Buf() for _ in range(2)]
            binter = [Buf() for _ in range(FT)]
            bys = Buf()
            self.norm_stats(xs, xb, subt)
            self.norm_apply(xs, xb, hs, hb, subt, gi)
            for ft in range(FT):
                bi = ft % NB
                P.dma("pool", lambda e, ft=ft, bi=bi: e.dma_start(out=wgs[bi][:], in_=wg[ft]), writes=[bwg[bi]])
                P.dma("pool", lambda e, ft=ft, bi=bi: e.dma_start(out=wus[bi][:], in_=wu[ft]), writes=[bwu[bi]])
                for si, (c0, w) in enumerate(subt):
                    P.op("pe", [lambda e, k=k, bi=bi, si=si, c0=c0, w=w: e.matmul(
                        self.ps[0 + si][:, 0:w], wgs[bi][:, k, :], hs[:, k, c0:c0 + w], start=(k == 0), stop=(k == KC - 1))
                        for k in range(KC)], reads=[bwg[bi], hb], writes=[self.bps[0 + si]])
                for si, (c0, w) in enumerate(subt):
                    P.op("pe", [lambda e, k=k, bi=bi, si=si, c0=c0, w=w: e.matmul(
                        self.ps[2 + si][:, 0:w], wus[bi][:, k, :], hs[:, k, c0:c0 + w], start=(k == 0), stop=(k == KC - 1))
                        for k in range(KC)], reads=[bwu[bi], hb], writes=[self.bps[2 + si]])
                sb_i = ft % 2
                for si, (c0, w) in enumerate(subt):
                    o0 = c0 - c00
                    P.op("act", lambda e, si=si, o0=o0, w=w, sb_i=sb_i: e.activation(
                        out=sil[sb_i][:, o0:o0 + w], in_=self.ps[0 + si][:, 0:w], func=AF.Silu),
                        reads=[self.bps[0 + si]], writes=[bsil[sb_i]])
                for si, (c0, w) in enumerate(subt):
                    o0 = c0 - c00
                    P.op("dve", lambda e, si=si, o0=o0, w=w, sb_i=sb_i, ft=ft: e.tensor_tensor(
                        out=inter[:, ft, o0:o0 + w], in0=self.ps[2 + si][:, 0:w], in1=sil[sb_i][:, o0:o0 + w], op=ALU.mult),
                        reads=[self.bps[2 + si], bsil[sb_i]], writes=[binter[ft]])
            for o in range(KC):
                bi = o % 2
                P.dma("pool", lambda e, o=o, bi=bi: e.dma_start(out=wds[bi][:], in_=wd[o]), writes=[bwd[bi]])
                for si, (c0, w) in enumerate(subt):
                    o0 = c0 - c00
                    pb = 4 + 2 * bi + si
                    P.op("pe", [lambda e, f=f, bi=bi, pb=pb, o0=o0, w=w: e.matmul(
                        self.ps[pb][:, 0:w], wds[bi][:, f, :], inter[:, f, o0:o0 + w], start=(f == 0), stop=(f == FT - 1))
                        for f in range(FT)], reads=[bwd[bi]] + binter, writes=[self.bps[pb]])
                    P.op("act", lambda e, pb=pb, o=o, o0=o0, w=w: e.activation(
                        out=ys[:, o, o0:o0 + w], in_=self.ps[pb][:, 0:w], func=AF.Copy),
                        reads=[self.bps[pb]], writes=[bys])
            self.post_norm_residual(ys, bys, xs, xb, subt, gi + 1, 0.5)
            P.fence()

    def post_norm_residual(self, ys, bys, xs, xb, subt, gidx, scale):
        P = self.P
        W = sum(w for _, w in subt)
        c00 = subt[0][0]
        ysub = [(c0 - c00, w) for (c0, w) in subt]
        self.norm_stats(ys, bys, ysub, scale_extra=scale)
        for c in range(KC):
            P.op("dve", lambda e, c=c: e.scalar_tensor_tensor(
                out=ys[:, c, :], in0=ys[:, c, :], scalar=self.gsb[:, gidx * KC + c:gidx * KC + c + 1],
                in1=self.rstd[:, 0:W], op0=ALU.mult, op1=ALU.mult),
                reads=[bys, self.brstd, self.bconst], writes=[bys])
            P.op("dve", lambda e, c=c: e.tensor_tensor(out=xs[:, c, c00:c00 + W], in0=xs[:, c, c00:c00 + W],
                                                       in1=ys[:, c, :], op=ALU.add),
                 reads=[bys, xb], writes=[xb])

    def gla_pass(self, xs, xb, subt, Wd, mixT, bmixT, last, pidx):
        P = self.P
        W = sum(w for _, w in subt)
        c00 = subt[0][0]
        tiles = [(subt[0][0] + 128 * i, 128) for i in range(subt[0][1] // 128)]
        if len(subt) > 1:
            tiles.append((subt[1][0], 1))
        NT = len(tiles)
        has_smp = len(subt) > 1
        with ExitStack() as st1:
            qT = self.sb(st1, "qT", [96, 8, W], BF16)
            kT = self.sb(st1, "kT", [96, 8, W], BF16)
            qmT = self.sb(st1, "qmT", [128, 4, W], BF16)
            ktm = self.sb(st1, "ktm", [128, NT, 768], BF16)
            vtm = self.sb(st1, "vtm", [128, NT, 1536], BF16)
            rs = self.sb(st1, "rs", [128, NT, 1536], BF16)
            aT = self.sb(st1, "aT", [32, W], F32)
            bqT, bkT, bqmT, baT = Buf(), Buf(), Buf(), Buf()
            bk = [Buf() for _ in range(NT)]
            bv = [Buf() for _ in range(NT)]
            br = [Buf() for _ in range(NT)]
            with ExitStack() as st:
                hs = self.sb(st, "hsg", [128, KC, 513], BF16)
                hb = Buf("h")
                wsl = [self.sb(st, "wsl%d" % i, [128, KC, 384], BF16) for i in range(2)]
                bwsl = [Buf() for _ in range(2)]
                wfm = [self.sb(st, "wfm%d" % i, [128, KC, 128], BF16) for i in range(2)]
                bwfm = [Buf() for _ in range(2)]
                was = self.sb(st, "was", [128, KC, 16], BF16)
                bwas = Buf()
                self.norm_stats(xs, xb, subt)
                self.norm_apply(xs, xb, hs, hb, subt, 2)
                P.dma("pool", lambda e: e.dma_start(out=was[:], in_=Wd["wa"]), writes=[bwas])
                P.op("dve", lambda e: e.memset(aT[:, :], 1.0), writes=[baT])
                for si, (c0, w) in enumerate(subt):
                    o0 = c0 - c00
                    P.op("pe", [lambda e, k=k, si=si, c0=c0, w=w: e.matmul(
                        self.ps[6 + si][0:16, 0:w], was[:, k, :], hs[:, k, c0:c0 + w], start=(k == 0), stop=(k == KC - 1))
                        for k in range(KC)], reads=[bwas, hb], writes=[self.bps[6 + si]])
                    P.op("act", lambda e, si=si, o0=o0, w=w: e.activation(out=aT[0:16, o0:o0 + w], in_=self.ps[6 + si][0:16, 0:w],
                                                                           func=AF.Copy),
                         reads=[self.bps[6 + si]], writes=[baT])
                fm_jobs = [("wq", ht, 96, qT, bqT, 192.0 ** -0.5) for ht in range(8)]
                fm_jobs += [("wk", ht, 96, kT, bkT, 1.0) for ht in range(8)]
                fm_jobs += [("wqm", h, 128, qmT, bqmT, 128.0 ** -0.5) for h in range(4)]
                pbi = 0
                for ji, (wn, idx, M, dst, bdst, scl) in enumerate(fm_jobs):
                    bi = ji % 2
                    P.dma("pool", lambda e, wn=wn, idx=idx, bi=bi, M=M: e.dma_start(out=wfm[bi][:, :, 0:M], in_=Wd[wn][idx]),
                          writes=[bwfm[bi]])
                    for si, (c0, w) in enumerate(subt):
                        o0 = c0 - c00
                        pb = pbi % 4
                        pbi += 1
                        P.op("pe", [lambda e, k=k, bi=bi, pb=pb, c0=c0, w=w, M=M: e.matmul(
                            self.ps[pb][0:M, 0:w], wfm[bi][:, k, 0:M], hs[:, k, c0:c0 + w], start=(k == 0), stop=(k == KC - 1))
                            for k in range(KC)], reads=[bwfm[bi], hb], writes=[self.bps[pb]])
                        P.op("act", lambda e, pb=pb, idx=idx, o0=o0, w=w, M=M, dst=dst, scl=scl: e.mul(
                            dst[0:M, idx, o0:o0 + w], self.ps[pb][0:M, 0:w], scl),
                            reads=[self.bps[pb]], writes=[bdst])
                for sl in range(10):
                    bi = sl % 2
                    P.dma("pool", lambda e, sl=sl, bi=bi: e.dma_start(out=wsl[bi][:], in_=Wd["wtm"][sl]), writes=[bwsl[bi]])
                    for ti, (t0, nt) in enumerate(tiles):
                        pb = 4 + (pbi % 4)
                        pbi += 1
                        P.op("pe", [lambda e, k=k, bi=bi, pb=pb, t0=t0, nt=nt: e.matmul(
                            self.ps[pb][0:nt, 0:384], hs[:, k, t0:t0 + nt], wsl[bi][:, k, :], start=(k == 0), stop=(k == KC - 1))
                            for k in range(KC)], reads=[bwsl[bi], hb], writes=[self.bps[pb]])
                        if sl < 2:
                            P.op("act", lambda e, pb=pb, ti=ti, nt=nt, sl=sl: e.activation(
                                out=ktm[0:nt, ti, sl * 384:(sl + 1) * 384], in_=self.ps[pb][0:nt, 0:384], func=AF.Copy),
                                reads=[self.bps[pb]], writes=[bk[ti]])
                        elif sl < 6:
                            P.op("dve", lambda e, pb=pb, ti=ti, nt=nt, sl=sl: e.tensor_copy(
                                out=vtm[0:nt, ti, (sl - 2) * 384:(sl - 1) * 384], in_=self.ps[pb][0:nt, 0:384]),
                                reads=[self.bps[pb]], writes=[bv[ti]])
                        else:
                            P.op("act", lambda e, pb=pb, ti=ti, nt=nt, sl=sl: e.activation(
                                out=rs[0:nt, ti, (sl - 6) * 384:(sl - 5) * 384], in_=self.ps[pb][0:nt, 0:384], func=AF.Silu),
                                reads=[self.bps[pb]], writes=[br[ti]])
                P.fence()
            with ExitStack() as st:
                sp = self.sb(st, "sp", [128, 768], F32)
                bcs = self.sb(st, "bcs", [128, 768], F32)
                kd = self.sb(st, "kd", [128, 768], BF16)
                dec = self.sb(st, "dec", [96, 8], F32)
                eqk = [self.sb(st, "eqk%d" % i, [96, 128], F32) for i in range(4)]
                qeT = self.sb(st, "qeT", [96, 8, 128], BF16)
                keT = self.sb(st, "keT", [96, 8, 128], BF16)
                ATm = self.sb(st, "ATm", [128, 4, 128], BF16)
                osb = [self.sb(st, "osb%d" % i, [128, 384], F32) for i in range(2)]
                sqh = self.sb(st, "sqh", [128, 384], F32)
                ss = self.sb(st, "ss", [128, 8], F32)
                mix = self.sb(st, "mix", [128, 2048], F32)
                pp = self.sb(st, "pp", [128, 256], F32)
                pT = self.sb(st, "pT", [128, 2, 128], BF16)
                mst = self.sb(st, "mst", [128, 8], F32)
                bsp, bbcs, bkd, bdec = Buf(), Buf(), Buf(), Buf()
                beqk = [Buf() for _ in range(4)]
                bqe, bke, bATm, bsqh, bss, bmix, bpp, bpT, bmst = Buf(), Buf(), Buf(), Buf(), Buf(), Buf(), Buf(), Buf(), Buf()
                bosb = [Buf(), Buf()]
                S = self.sb(st, "S", [96, 8, 384], F32)
                Sb = self.sb(st, "Sb", [96, 8, 384], BF16)
                mKp = self.sb(st, "mKp", [128, 4, 256], BF16)
                mVp = self.sb(st, "mVp", [128, 2, 512], BF16)
                bS, bSb, bmemp = Buf(), Buf(), Buf()
                P.dma("sp", lambda e: e.dma_start(out=S[:], in_=self.Sscr), reads=[self.bSscr], writes=[bS])
                P.op("dve", lambda e: e.tensor_scalar(S[:, :, :], S[:, :, :], self.keepv[0:96, pidx:pidx + 1], None, op0=ALU.mult),
                     reads=[bS, self.bconst], writes=[bS])
                P.op("act", lambda e: e.activation(out=Sb[:, :, :], in_=S[:, :, :], func=AF.Copy), reads=[bS], writes=[bSb])
                P.dma("pool", lambda e: e.dma_start(out=mKp[:], in_=self.memK_d[0]), reads=[self.bmemd], writes=[bmemp])
                P.dma("pool", lambda e: e.dma_start(out=mVp[:], in_=self.memV_d[0]), reads=[self.bmemd], writes=[bmemp])
                tts = [(t0, nt, S, bS, Sb, bSb, (mKp, mVp, bmemp)) for (t0, nt) in tiles if nt == 128]
                if has_smp:
                    Ss = self.sb(st, "Ss", [96, 8, 384], F32)
                    Ssb = self.sb(st, "Ssb", [96, 8, 384], BF16)
                    mKs = self.sb(st, "mKs", [128, 4, 256], BF16)
                    mVs = self.sb(st, "mVs", [128, 2, 512], BF16)
                    bSs, bSsb, bmems = Buf(), Buf(), Buf()
                    P.dma("sp", lambda e: e.dma_start(out=Ss[:], in_=self.sgla), writes=[bSs])
                    P.op("act", lambda e: e.activation(out=Ssb[:, :, :], in_=Ss[:, :, :], func=AF.Copy), reads=[bSs], writes=[bSsb])
                    P.dma("pool", lambda e: e.dma_start(out=mKs[:], in_=self.cmk[0]), writes=[bmems])
                    P.dma("pool", lambda e: e.dma_start(out=mVs[:], in_=self.cmv[0]), writes=[bmems])
                    tts.append((tiles[-1][0], 1, Ss, bSs, Ssb, bSsb, (mKs, mVs, bmems)))
                for ti, (t0, nt, Sx, bSx, Sxb, bSxb, (mK, mV, bmem)) in enumerate(tts):
                    o0 = t0 - c00
                    for hh in range(2):
                        P.op("pe", lambda e, hh=hh, o0=o0, nt=nt: e.matmul(
                            self.ps[4 + hh][0:nt, 0:384], aT[0:17, o0:o0 + nt], self.wa2[0:17, hh * 384:(hh + 1) * 384],
                            start=True, stop=True), reads=[baT, self.bconst], writes=[self.bps[4 + hh]])
                        P.op("act", lambda e, hh=hh, nt=nt: e.activation(out=sp[0:nt, hh * 384:(hh + 1) * 384],
                                                                          in_=self.ps[4 + hh][0:nt, 0:384], func=AF.Exp, scale=-1.0),
                             reads=[self.bps[4 + hh]], writes=[bsp])
                    P.op("act", lambda e, nt=nt: e.activation(out=sp[0:nt, :], in_=sp[0:nt, :], func=AF.Ln, bias=1.0),
                         reads=[bsp], writes=[bsp])
                    for hh in range(2):
                        P.op("pe", lambda e, hh=hh, nt=nt: e.matmul(
                            self.ps[4 + hh][0:nt, 0:384], self.ones[0:nt, 0:nt], sp[0:nt, hh * 384:(hh + 1) * 384],
                            start=True, stop=True), reads=[bsp, self.bconst], writes=[self.bps[4 + hh]])
                        P.op("pe", lambda e, hh=hh, nt=nt: e.matmul(
                            self.ps[6 + hh][0:nt, 0:384], self.utri[0:nt, 0:nt], sp[0:nt, hh * 384:(hh + 1) * 384],
                            start=True, stop=True), reads=[bsp, self.bconst], writes=[self.bps[6 + hh]])
                        P.op("act", lambda e, hh=hh, nt=nt: e.activation(out=bcs[0:nt, hh * 384:(hh + 1) * 384],
                                                                          in_=self.ps[6 + hh][0:nt, 0:384], func=AF.Copy),
                             reads=[self.bps[6 + hh]], writes=[bbcs])
                        P.op("dve", lambda e, hh=hh, nt=nt: e.tensor_tensor(
                            out=bcs[0:nt, hh * 384:(hh + 1) * 384], in0=self.ps[4 + hh][0:nt, 0:384],
                            in1=bcs[0:nt, hh * 384:(hh + 1) * 384], op=ALU.subtract),
                            reads=[self.bps[4 + hh], bbcs], writes=[bbcs])
                    P.op("act", lambda e, nt=nt: e.activation(out=bcs[0:nt, :], in_=bcs[0:nt, :], func=AF.Exp, scale=-1.0 / 16.0),
                         reads=[bbcs], writes=[bbcs])
                    P.op("dve", lambda e, nt=nt, ti=ti: e.tensor_tensor(out=kd[0:nt, :], in0=ktm[0:nt, ti, :], in1=bcs[0:nt, :],
                                                                        op=ALU.mult),
                         reads=[bbcs, bk[ti]], writes=[bkd])
                    P.op("pe", [lambda e, ht=ht, nt=nt: e.matmul(
                        self.ps[2][0:96, ht:ht + 1], sp[0:nt, ht * 96:(ht + 1) * 96], self.ones[0:nt, 0:1], start=True, stop=True)
                        for ht in range(8)], reads=[bsp, self.bconst], writes=[self.bps[2]])
                    P.op("act", lambda e: e.activation(out=dec[:, :], in_=self.ps[2][0:96, 0:8], func=AF.Exp, scale=-1.0 / 16.0),
                         reads=[self.bps[2]], writes=[bdec])
                    for ht in range(8):
                        pb = ht // 4
                        cc = (ht % 4) * 128
                        P.op("pe", lambda e, ht=ht, pb=pb, cc=cc, nt=nt: e.matmul(
                            self.ps[pb][0:96, cc:cc + nt], sp[0:nt, ht * 96:(ht + 1) * 96], self.utri[0:nt, 0:nt],
                            start=True, stop=True), reads=[bsp, self.bconst], writes=[self.bps[pb]])
                        e0, e1 = (2 * ht) % 4, (2 * ht + 1) % 4
                        P.op("act", lambda e, pb=pb, cc=cc, nt=nt, e0=e0: e.activation(
                            out=eqk[e0][:, 0:nt], in_=self.ps[pb][0:96, cc:cc + nt], func=AF.Exp, scale=-1.0 / 16.0),
                            reads=[self.bps[pb]], writes=[beqk[e0]])
                        P.op("act", lambda e, pb=pb, cc=cc, nt=nt, e1=e1: e.activation(
                            out=eqk[e1][:, 0:nt], in_=self.ps[pb][0:96, cc:cc + nt], func=AF.Exp, scale=1.0 / 16.0),
                            reads=[self.bps[pb]], writes=[beqk[e1]])
                        P.op("dve", lambda e, ht=ht, o0=o0, nt=nt, e0=e0: e.tensor_tensor(
                            out=qeT[:, ht, 0:nt], in0=qT[:, ht, o0:o0 + nt], in1=eqk[e0][:, 0:nt], op=ALU.mult),
                            reads=[bqT, beqk[e0]], writes=[bqe])
                        P.op("dve", lambda e, ht=ht, o0=o0, nt=nt, e1=e1: e.tensor_tensor(
                            out=keT[:, ht, 0:nt], in0=kT[:, ht, o0:o0 + nt], in1=eqk[e1][:, 0:nt], op=ALU.mult),
                            reads=[bkT, beqk[e1]], writes=[bke])
                    for h in range(4):
                        P.op("pe", [lambda e, h=h, kt=kt, nt=nt: e.matmul(
                            self.ps[3][0:nt, h * 128:h * 128 + nt], keT[:, 2 * h + kt, 0:nt], qeT[:, 2 * h + kt, 0:nt],
                            start=(kt == 0), stop=(kt == 1)) for kt in range(2)], reads=[bqe, bke], writes=[self.bps[3]])
                        P.op("dve", lambda e, h=h, nt=nt: e.tensor_tensor(
                            out=ATm[0:nt, h, 0:nt], in0=self.ps[3][0:nt, h * 128:h * 128 + nt], in1=self.utri[0:nt, 0:nt],
                            op=ALU.mult), reads=[self.bps[3], self.bconst], writes=[bATm])
                    for h in range(4):
                        pb = 4 + h
                        ob = h % 2
                        P.op("pe", [
                            lambda e, h=h, pb=pb, nt=nt, Sxb=Sxb: e.matmul(self.ps[pb][0:nt, 0:384], qeT[:, 2 * h, 0:nt],
                                                                         Sxb[:, 2 * h, :], start=True, stop=False),
                            lambda e, h=h, pb=pb, nt=nt, Sxb=Sxb: e.matmul(self.ps[pb][0:nt, 0:384], qeT[:, 2 * h + 1, 0:nt],
                                                                         Sxb[:, 2 * h + 1, :], start=False, stop=False),
                            lambda e, h=h, pb=pb, nt=nt, ti=ti: e.matmul(self.ps[pb][0:nt, 0:384], ATm[0:nt, h, 0:nt],
                                                                       vtm[0:nt, ti, h * 384:(h + 1) * 384], start=False, stop=True)],
                            reads=[bqe, bSxb, bATm, bv[ti]], writes=[self.bps[pb]])
                        P.op("act", lambda e, pb=pb, nt=nt, ob=ob: e.activation(out=osb[ob][0:nt, :], in_=self.ps[pb][0:nt, 0:384],
                                                                                 func=AF.Copy),
                             reads=[self.bps[pb]], writes=[bosb[ob]])
                        P.op("act", lambda e, pb=pb, nt=nt: e.activation(out=sqh[0:nt, :], in_=self.ps[pb][0:nt, 0:384],
                                                                          func=AF.Square),
                             reads=[self.bps[pb]], writes=[bsqh])
                        P.op("dve", lambda e, h=h, nt=nt: e.reduce_sum(out=ss[0:nt, h:h + 1], in_=sqh[0:nt, :], axis=AX.X),
                             reads=[bsqh], writes=[bss])
                        P.op("act", lambda e, h=h, nt=nt: e.activation(out=ss[0:nt, h:h + 1], in_=ss[0:nt, h:h + 1], func=AF.Ln,
                                                                        scale=1.0 / 384.0, bias=EPS), reads=[bss], writes=[bss])
                        P.op("act", lambda e, h=h, nt=nt: e.activation(out=ss[0:nt, h:h + 1], in_=ss[0:nt, h:h + 1], func=AF.Exp,
                                                                        scale=-0.5), reads=[bss], writes=[bss])
                        P.op("dve", lambda e, h=h, nt=nt, ob=ob: e.scalar_tensor_tensor(
                            out=osb[ob][0:nt, :], in0=osb[ob][0:nt, :], scalar=ss[0:nt, h:h + 1], in1=self.gon[0:nt, :],
                            op0=ALU.mult, op1=ALU.mult), reads=[bosb[ob], bss, self.bconst], writes=[bosb[ob]])
                        P.op("dve", lambda e, h=h, nt=nt, ob=ob, ti=ti: e.tensor_tensor(
                            out=mix[0:nt, h * 384:(h + 1) * 384], in0=osb[ob][0:nt, :], in1=rs[0:nt, ti, h * 384:(h + 1) * 384],
                            op=ALU.mult), reads=[bosb[ob], br[ti]], writes=[bmix])
                    for ht in range(8):
                        h = ht // 2
                        pb = ht % 3
                        P.op("pe", lambda e, ht=ht, h=h, pb=pb, nt=nt, ti=ti: e.matmul(
                            self.ps[pb][0:96, 0:384], kd[0:nt, ht * 96:(ht + 1) * 96], vtm[0:nt, ti, h * 384:(h + 1) * 384],
                            start=True, stop=True), reads=[bkd, bv[ti]], writes=[self.bps[pb]])
                        P.op("dve", lambda e, ht=ht, pb=pb, Sx=Sx: e.scalar_tensor_tensor(
                            out=Sx[:, ht, :], in0=Sx[:, ht, :], scalar=dec[:, ht:ht + 1], in1=self.ps[pb][0:96, 0:384],
                            op0=ALU.mult, op1=ALU.add), reads=[bdec, self.bps[pb], bSx], writes=[bSx])
                        P.op("act", lambda e, ht=ht, Sx=Sx, Sxb=Sxb: e.activation(out=Sxb[:, ht, :], in_=Sx[:, ht, :], func=AF.Copy),
                             reads=[bSx], writes=[bSxb])
                    for h in range(4):
                        pb = 4 + (h % 2) * 2
                        P.op("pe", lambda e, h=h, pb=pb, o0=o0, nt=nt, mK=mK: e.matmul(
                            self.ps[pb][0:nt, 0:256], qmT[:, h, o0:o0 + nt], mK[:, h, :], start=True, stop=True),
                            reads=[bqmT, bmem], writes=[self.bps[pb]])
                        P.op("dve", lambda e, pb=pb, nt=nt, h=h: e.reduce_max(out=mst[0:nt, 0:1], in_=self.ps[pb][0:nt, 0:256], axis=AX.X),
                             reads=[self.bps[pb]], writes=[bmst])
                        P.op("dve", lambda e, nt=nt: e.tensor_scalar(mst[0:nt, 0:1], mst[0:nt, 0:1], -1.0, None, op0=ALU.mult),
                             reads=[bmst], writes=[bmst])
                        P.op("act", lambda e, pb=pb, nt=nt: e.activation(out=pp[0:nt, :], in_=self.ps[pb][0:nt, 0:256], func=AF.Exp,
                                                                          bias=mst[0:nt, 0:1]),
                             reads=[self.bps[pb], bmst], writes=[bpp])
                        P.op("dve", lambda e, nt=nt: e.reduce_sum(out=mst[0:nt, 1:2], in_=pp[0:nt, :], axis=AX.X),
                             reads=[bpp, bmst], writes=[bmst])
                        P.op("dve", lambda e, nt=nt: e.reciprocal(mst[0:nt, 1:2], mst[0:nt, 1:2]), reads=[bmst], writes=[bmst])
                        for mt in range(2):
                            P.op("pe", lambda e, pb=pb, mt=mt, nt=nt: e.transpose(
                                self.ps[pb + 1][0:128, mt * 128:mt * 128 + nt], pp[0:nt, mt * 128:(mt + 1) * 128],
                                self.ident[0:nt, 0:nt]), reads=[bpp, self.bconst], writes=[self.bps[pb + 1]])
                        P.op("act", [lambda e, pb=pb, nt=nt, mt=mt: e.activation(
                            out=pT[:, mt, 0:nt], in_=self.ps[pb + 1][0:128, mt * 128:mt * 128 + nt],
                            func=AF.Copy) for mt in range(2)], reads=[self.bps[pb + 1]], writes=[bpT])
                        P.op("pe", [lambda e, pb=pb, mt=mt, nt=nt, h=h, mV=mV: e.matmul(
                            self.ps[pb][0:nt, 256:384], pT[:, mt, 0:nt], mV[:, mt, h * 128:(h + 1) * 128],
                            start=(mt == 0), stop=(mt == 1)) for mt in range(2)], reads=[bpT, bmem], writes=[self.bps[pb]])
                        P.op("dve", lambda e, pb=pb, nt=nt, h=h: e.tensor_scalar(
                            mix[0:nt, 1536 + h * 128:1536 + (h + 1) * 128], self.ps[pb][0:nt, 256:384], mst[0:nt, 1:2], None,
                            op0=ALU.mult), reads=[self.bps[pb], bmst], writes=[bmix])
                    for fc in range(KC):
                        pb = (fc // 4) % 4
                        cc = (fc % 4) * 128
                        P.op("pe", lambda e, fc=fc, pb=pb, cc=cc, nt=nt: e.transpose(
                            self.ps[pb][0:128, cc:cc + nt], mix[0:nt, fc * 128:(fc + 1) * 128], self.ident[0:nt, 0:nt]),
                            reads=[bmix, self.bconst], writes=[self.bps[pb]])
                        eng = "act" if fc % 2 == 0 else "dve"
                        if eng == "act":
                            P.op("act", lambda e, fc=fc, pb=pb, cc=cc, nt=nt, o0=o0: e.activation(
                                out=mixT[:, fc, o0:o0 + nt], in_=self.ps[pb][0:128, cc:cc + nt], func=AF.Copy),
                                reads=[self.bps[pb]], writes=[bmixT])
                        else:
                            P.op("dve", lambda e, fc=fc, pb=pb, cc=cc, nt=nt, o0=o0: e.tensor_copy(
                                out=mixT[:, fc, o0:o0 + nt], in_=self.ps[pb][0:128, cc:cc + nt]),
                                reads=[self.bps[pb]], writes=[bmixT])
                P.dma("sp", lambda e: e.dma_start(out=self.Sscr, in_=S[:]), reads=[bS], writes=[self.bSscr])
                if last:
                    P.dma("sp", lambda e: e.dma_start(out=self.o_gp, in_=S[:]), reads=[bS], writes=[self.bout])
                if has_smp:
                    P.dma("sp", lambda e: e.dma_start(out=self.o_gs, in_=Ss[:]), reads=[bSs], writes=[self.bout])
                P.fence()

    def wout_pass(self, xs, xb, subt, wo, mixT, bmixT, gidx):
        P = self.P
        W = sum(w for _, w in subt)
        c00 = subt[0][0]
        with ExitStack() as st:
            ys = self.sb(st, "ysw", [128, KC, W], F32)
            wos = [self.sb(st, "wo%d" % i, [128, KC, 128], BF16) for i in range(2)]
            bwo = [Buf(), Buf()]
            bys = Buf()
            for o in range(KC):
                bi = o % 2
                P.dma("pool", lambda e, o=o, bi=bi: e.dma_start(out=wos[bi][:], in_=wo[o]), writes=[bwo[bi]])
                for si, (c0, w) in enumerate(subt):
                    o0 = c0 - c00
                    pb = 4 + 2 * bi + si
                    P.op("pe", [lambda e, f=f, bi=bi, pb=pb, o0=o0, w=w: e.matmul(
                        self.ps[pb][:, 0:w], wos[bi][:, f, :], mixT[:, f, o0:o0 + w], start=(f == 0), stop=(f == KC - 1))
                        for f in range(KC)], reads=[bwo[bi], bmixT], writes=[self.bps[pb]])
                    P.op("act", lambda e, pb=pb, o=o, o0=o0, w=w: e.activation(
                        out=ys[:, o, o0:o0 + w], in_=self.ps[pb][:, 0:w], func=AF.Copy),
                        reads=[self.bps[pb]], writes=[bys])
            self.post_norm_residual(ys, bys, xs, xb, subt, gidx, 1.0)
            P.fence()

    def mem_kv_phase(self, memT, wmem, mg, o_mem, wmemk):
        P = self.P
        with ExitStack() as st:
            xm = self.sb(st, "xm", [128, KC, 256], F32)
            hm = self.sb(st, "hm", [128, KC, 256], BF16)
            wm = [self.sb(st, "wm%d" % i, [128, KC, 512], BF16) for i in range(2)]
            om = [self.sb(st, "om%d" % i, [128, 512], F32) for i in range(2)]
            bxm, bhm = Buf(), Buf()
            bwm = [Buf(), Buf()]
            bom = [Buf(), Buf()]
            bo = Buf()
            P.dma("sp", lambda e: e.dma_start(out=xm[:], in_=memT.rearrange("(c p) t -> p c t", p=128)), writes=[bxm])
            self.norm_stats(xm, bxm, [(0, 256)])
            cnt = 0
            for i in range(2):
                for c in range(KC):
                    P.op("dve", lambda e, c=c, i=i: e.scalar_tensor_tensor(
                        out=hm[:, c, :], in0=xm[:, c, :], scalar=mg[:, i * KC + c:i * KC + c + 1], in1=self.rstd[:, 0:256],
                        op0=ALU.mult, op1=ALU.mult), reads=[bxm, self.brstd, self.bconst], writes=[bhm])
                for sl in range(2):
                    bi = cnt % 2
                    P.dma("pool", lambda e, i=i, sl=sl, bi=bi: e.dma_start(out=wm[bi][:], in_=wmem[i, sl]), writes=[bwm[bi]])
                    for tt in range(2):
                        pb = cnt % 4
                        ob = cnt % 2
                        cnt += 1
                        P.op("pe", [lambda e, k=k, bi=bi, pb=pb, tt=tt: e.matmul(
                            self.ps[pb][:, 0:512], hm[:, k, tt * 128:(tt + 1) * 128], wm[bi][:, k, :],
                            start=(k == 0), stop=(k == KC - 1)) for k in range(KC)],
                            reads=[bwm[bi], bhm], writes=[self.bps[pb]])
                        P.op("act", lambda e, pb=pb, ob=ob: e.activation(out=om[ob][:, :], in_=self.ps[pb][:, 0:512], func=AF.Copy),
                             reads=[self.bps[pb]], writes=[bom[ob]])
                        P.dma("sp", lambda e, i=i, sl=sl, tt=tt, ob=ob: e.dma_start(
                            out=o_mem[i, tt * 128:(tt + 1) * 128, sl * 512:(sl + 1) * 512], in_=om[ob][:, :]),
                            reads=[bom[ob]], writes=[bo])
                        if sl == 1:
                            P.dma("sp", lambda e, i=i, tt=tt, ob=ob: e.dma_start(
                                out=self.memV_d[i][:, tt, :], in_=om[ob][:, :]), reads=[bom[ob]], writes=[self.bmemd])
                for h in range(4):
                    bi = cnt % 2
                    pb = cnt % 4
                    ob = cnt % 2
                    cnt += 1
                    P.dma("pool", lambda e, i=i, h=h, bi=bi: e.dma_start(out=wm[bi][:, :, 0:128], in_=wmemk[i, h]), writes=[bwm[bi]])
                    P.op("pe", [lambda e, k=k, bi=bi, pb=pb: e.matmul(
                        self.ps[pb][:, 0:256], wm[bi][:, k, 0:128], hm[:, k, 0:256],
                        start=(k == 0), stop=(k == KC - 1)) for k in range(KC)],
                        reads=[bwm[bi], bhm], writes=[self.bps[pb]])
                    P.op("act", lambda e, pb=pb, ob=ob: e.activation(out=om[ob][:, 0:256], in_=self.ps[pb][:, 0:256], func=AF.Copy),
                         reads=[self.bps[pb]], writes=[bom[ob]])
                    P.dma("sp", lambda e, i=i, h=h, ob=ob: e.dma_start(out=self.memK_d[i][:, h, :], in_=om[ob][:, 0:256]),
                          reads=[bom[ob]], writes=[self.bmemd])
            P.fence()

    def nsa_kv_pass(self, xs, xb, subt, wkvn, o_kv, tbase, swin, o_win_s, N=None, own_off=None):
        P = self.P
        tiles = [(subt[0][0] + 128 * i, 128) for i in range(subt[0][1] // 128)]
        if len(subt) > 1:
            tiles.append((subt[1][0], 1))
        with ExitStack() as st:
            hs = self.sb(st, "hsn", [128, KC, 513], BF16)
            hb = Buf("h")
            wsl = [self.sb(st, "wsn%d" % i, [128, KC, 384], BF16) for i in range(2)]
            bwsl = [Buf(), Buf()]
            ot = [self.sb(st, "otn%d" % i, [128, 384], F32) for i in range(3)]
            bot = [Buf() for _ in range(3)]
            self.norm_stats(xs, xb, subt)
            self.norm_apply(xs, xb, hs, hb, subt, 6 + 2)
            cnt = 0
            for sl in range(6):
                bi = sl % 2
                P.dma("pool", lambda e, sl=sl, bi=bi: e.dma_start(out=wsl[bi][:], in_=wkvn[sl]), writes=[bwsl[bi]])
                for ti, (t0, nt) in enumerate(tiles):
                    pb = cnt % 4
                    ob = cnt % 3
                    cnt += 1
                    P.op("pe", [lambda e, k=k, bi=bi, pb=pb, t0=t0, nt=nt: e.matmul(
                        self.ps[pb][0:nt, 0:384], hs[:, k, t0:t0 + nt], wsl[bi][:, k, :], start=(k == 0), stop=(k == KC - 1))
                        for k in range(KC)], reads=[bwsl[bi], hb], writes=[self.bps[pb]])
                    P.op("act", lambda e, pb=pb, ob=ob, nt=nt: e.activation(out=ot[ob][0:nt, :], in_=self.ps[pb][0:nt, 0:384],
                                                                             func=AF.Copy), reads=[self.bps[pb]], writes=[bot[ob]])
                    P.dma("sp", lambda e, ob=ob, nt=nt, t0=t0, sl=sl: e.dma_start(
                        out=o_kv[tbase + t0:tbase + t0 + nt, sl * 384:(sl + 1) * 384], in_=ot[ob][0:nt, :]),
                        reads=[bot[ob]], writes=[self.bout])
                    if nt == 1 and sl >= 4:
                        P.dma("sp", lambda e, ob=ob, sl=sl: e.dma_start(
                            out=o_win_s[511:512, (sl - 4) * 384:(sl - 3) * 384], in_=ot[ob][0:1, :]),
                            reads=[bot[ob]], writes=[self.bout])
            if len(subt) > 1:
                P.dma("sp", lambda e: e.dma_start(out=o_win_s[0:511, :], in_=swin[1:512, :]), writes=[self.bout])
            if N is not None:
                c00 = subt[0][0]
                Wp = subt[0][1]
                wfm = [self.sb(st, "wfn%d" % i, [128, KC, 128], BF16) for i in range(2)]
                bwfm = [Buf(), Buf()]
                stg_ = [self.sb(st, "stgn%d" % i, [128, 512], F32) for i in range(2)]
                bstg = [Buf(), Buf()]
                jobs = [(i, N["kT_d"][i // 3, i % 3], tbase, 1.0) for i in range(12)]
                if own_off is not None:
                    jobs += [(12 + h, N["qT_d"][h], own_off, 128.0 ** -0.5) for h in range(12)]
                    jobs += [(24 + h, N["qmT_d"][h], own_off, 128.0 ** -0.5) for h in range(4)]
                for ji, (wi, dst, off, scl) in enumerate(jobs):
                    bi = ji % 2
                    pb = 4 + ji % 4
                    P.dma("pool", lambda e, wi=wi, bi=bi: e.dma_start(out=wfm[bi][:], in_=N["wnfm"][wi]), writes=[bwfm[bi]])
                    P.op("pe", [lambda e, k=k, bi=bi, pb=pb: e.matmul(self.ps[pb][:, 0:Wp], wfm[bi][:, k, :], hs[:, k, c00:c00 + Wp],
                                                                      start=(k == 0), stop=(k == KC - 1)) for k in range(KC)],
                         reads=[bwfm[bi], hb], writes=[self.bps[pb]])
                    P.op("act", lambda e, pb=pb, bi=bi, scl=scl: e.mul(stg_[bi][:, 0:Wp], self.ps[pb][:, 0:Wp], scl),
                         reads=[self.bps[pb]], writes=[bstg[bi]])
                    P.dma("sp", lambda e, dst=dst, off=off, bi=bi: e.dma_start(out=dst[:, off + c00:off + c00 + Wp], in_=stg_[bi][:, 0:Wp]),
                          reads=[bstg[bi]], writes=[self.bnsa])
                    if len(subt) > 1 and (wi >= 12 or 6 <= wi < 12):
                        sc0 = subt[1][0]
                        if wi >= 24:
                            sdst = N["qmTs_d"][:, wi - 24:wi - 23]
                        elif wi >= 12:
                            sdst = N["qTs_d"][:, wi - 12:wi - 11]
                        else:
                            sdst = N["knew_d"][:, (wi - 6) // 3, (wi - 6) % 3:(wi - 6) % 3 + 1]
                        pb2 = (ji % 2)
                        sb2 = self.sstg[ji % 2]
                        bsb2 = self.bsstg[ji % 2]
                        P.op("pe", [lambda e, k=k, bi=bi, pb2=pb2, sc0=sc0: e.matmul(self.ps[pb2][:, 0:1], wfm[bi][:, k, :], hs[:, k, sc0:sc0 + 1],
                                                                                    start=(k == 0), stop=(k == KC - 1)) for k in range(KC)],
                             reads=[bwfm[bi], hb], writes=[self.bps[pb2]])
                        P.op("act", lambda e, pb2=pb2, sb2=sb2, scl=scl: e.mul(sb2[:, 0:1], self.ps[pb2][:, 0:1], scl),
                             reads=[self.bps[pb2]], writes=[bsb2])
                        P.dma("sp", lambda e, sdst=sdst, sb2=sb2: e.dma_start(out=sdst, in_=sb2[:, 0:1], allow_slow_non_contiguous=True),
                              reads=[bsb2], writes=[self.bnsa])
                if own_off is not None:
                    wgt = self.sb(st, "wgt", [128, KC, 36], BF16)
                    gbr = self.sb(st, "gbr", [1, 36], F32)
                    gst = [self.sb(st, "gst%d" % i, [128, 36], F32) for i in range(2)]
                    bwgt, bgst = Buf(), [Buf(), Buf()]
                    P.dma("pool", lambda e: e.dma_start(out=wgt[:], in_=N["wgate"]), writes=[bwgt])
                    P.dma("sp", lambda e: e.dma_start(out=gbr[:], in_=N["gate_b"]), writes=[bwgt])
                    for ti in range(Wp // 128):
                        t0 = c00 + ti * 128
                        pb = ti % 2
                        P.op("pe", [lambda e, k=k, pb=pb, t0=t0: e.matmul(self.ps[pb][:, 0:36], hs[:, k, t0:t0 + 128], wgt[:, k, :],
                                                                          start=(k == 0), stop=False) for k in range(KC)] +
                             [lambda e, pb=pb: e.matmul(self.ps[pb][:, 0:36], self.ones[0:1, 0:128], gbr[0:1, :], start=False, stop=True)],
                             reads=[bwgt, hb, self.bconst], writes=[self.bps[pb]])
                        P.op("act", lambda e, pb=pb: e.activation(out=gst[pb][:, :], in_=self.ps[pb][:, 0:36], func=AF.Sigmoid),
                             reads=[self.bps[pb]], writes=[bgst[pb]])
                        P.dma("sp", lambda e, pb=pb, t0=t0: e.dma_start(out=N["gates_d"][own_off + t0:own_off + t0 + 128, :], in_=gst[pb][:, :]),
                              reads=[bgst[pb]], writes=[self.bnsa])
                if len(subt) > 1 and own_off is not None:
                    sc0 = subt[1][0]
                    wgT = self.sb(st, "wgT", [128, 9, KC, 4], BF16)
                    gbT = self.sb(st, "gbT", [4, 9], F32)
                    gTt = self.sb(st, "gTt", [4, 9], F32)
                    bwgT, bgTt = Buf(), Buf()
                    P.dma("pool", lambda e: e.dma_start(out=wgT[:], in_=N["wgT"]), writes=[bwgT])
                    P.dma("sp", lambda e: e.dma_start(out=gbT[:], in_=N["gbT"]), writes=[bwgT])
                    for j9 in range(9):
                        P.op("pe", [lambda e, k=k, j9=j9, sc0=sc0: e.matmul(self.ps[2][0:4, j9:j9 + 1], wgT[:, j9, k, :], hs[:, k, sc0:sc0 + 1],
                                                                           start=(k == 0), stop=(k == KC - 1)) for k in range(KC)],
                             reads=[bwgT, hb], writes=[self.bps[2]])
                    P.op("dve", lambda e: e.tensor_tensor(out=gTt[:, :], in0=self.ps[2][0:4, 0:9], in1=gbT[:, :], op=ALU.add),
                         reads=[self.bps[2], bwgT], writes=[bgTt])
                    P.op("act", lambda e: e.activation(out=gTt[:, :], in_=gTt[:, :], func=AF.Sigmoid), reads=[bgTt], writes=[bgTt])
                    P.dma("sp", lambda e: e.dma_start(out=N["gT_d"], in_=gTt[:, :]), reads=[bgTt], writes=[self.bnsa])
            P.fence()

    def nsa_compress(self, st_out, A):
        P = self.P
        kcmpT = self.sb(st_out, "kcmpT", [128, 192], BF16)
        vcmp = self.sb(st_out, "vcmp", [64, 3, 128], BF16)
        bkc = Buf("kcmp")
        with ExitStack() as st:
            cmpT = self.sb(st, "cmpT", [128, 3, 4096], BF16)
            w1s = self.sb(st, "w1s", [128, 64, 256], BF16)
            w2s = self.sb(st, "w2s", [128, 2, 128], BF16)
            peT = self.sb(st, "peT", [128, 64], BF16)
            b1c = self.sb(st, "b1c", [128, 2], F32)
            hbc = self.sb(st, "hbc", [128, 2], F32)
            hidT = self.sb(st, "hidT", [128, 2, 192], BF16)
            bcm, bw, bhb, bhid = Buf(), Buf(), Buf(), Buf()
            for kv in range(2):
                P.dma("pool", lambda e, kv=kv: e.dma_start(out=cmpT[:], in_=A["kT"][kv].rearrange("g p t -> p g t")), writes=[bcm])
                P.dma("pool", lambda e, kv=kv: e.dma_start(out=w1s[:], in_=A["w1"][kv].rearrange("r p h -> p r h")), writes=[bw])
                P.dma("pool", lambda e, kv=kv: e.dma_start(out=w2s[:], in_=A["w2"][kv].rearrange("(t p) d -> p t d", p=128)), writes=[bw])
                P.dma("pool", lambda e, kv=kv: e.dma_start(out=peT[:], in_=A["peT"][kv]), writes=[bw])
                P.dma("sp", lambda e, kv=kv: e.dma_start(out=b1c[:], in_=A["b1c"][kv]), writes=[bw])
                for t in range(2):
                    P.op("pe", [lambda e, r=r, t=t: e.matmul(self.ps[3][:, t:t + 1], w1s[:, r, t * 128:(t + 1) * 128], peT[:, r:r + 1],
                                                             start=(r == 0), stop=(r == 63)) for r in range(64)],
                         reads=[bw], writes=[self.bps[3]])
                P.op("dve", lambda e: e.tensor_tensor(out=hbc[:, :], in0=self.ps[3][:, 0:2], in1=b1c[:, :], op=ALU.add),
                     reads=[self.bps[3], bw], writes=[bhb])
                for t in range(2):
                    P.op("pe", [lambda e, r=r, t=t: e.matmul(self.ps[t][:, 0:192], w1s[:, r, t * 128:(t + 1) * 128],
                                                             cmpT[:, :, r:4096:64], start=(r == 0), stop=(r == 63))
                                for r in range(64)], reads=[bw, bcm], writes=[self.bps[t]])
                    P.op("act", lambda e, t=t: e.activation(out=hidT[:, t, :], in_=self.ps[t][:, 0:192], func=AF.Silu,
                                                            bias=hbc[:, t:t + 1]), reads=[self.bps[t], bhb], writes=[bhid])
                if kv == 0:
                    P.op("pe", [lambda e, t=t: e.matmul(self.ps[2][:, 0:192], w2s[:, t, :], hidT[:, t, :], start=(t == 0), stop=(t == 1))
                                for t in range(2)], reads=[bw, bhid], writes=[self.bps[2]])
                    P.op("act", lambda e: e.activation(out=kcmpT[:, :], in_=self.ps[2][:, 0:192], func=AF.Copy),
                         reads=[self.bps[2]], writes=[bkc])
                else:
                    for g in range(3):
                        P.op("pe", [lambda e, t=t, g=g: e.matmul(self.ps[2][0:64, g * 128:(g + 1) * 128], hidT[:, t, g * 64:(g + 1) * 64],
                                                                 w2s[:, t, :], start=(t == 0), stop=(t == 1)) for t in range(2)],
                             reads=[bw, bhid], writes=[self.bps[2]])
                    P.op("act", lambda e: e.activation(out=vcmp[:, :, :], in_=self.ps[2][0:64, 0:384].rearrange("p (g d) -> p g d", g=3),
                                                       func=AF.Copy), reads=[self.bps[2]], writes=[bkc])
            P.fence()
        return kcmpT, vcmp, bkc

    def nsa_attn(self, A, qis, kcmpT, vcmp, bkc):
        P = self.P
        nq = len(qis)
        q0 = qis[0]
        with ExitStack() as st:
            qTs = self.sb(st, "qTs", [128, nq, 12, 128], BF16)
            qab = self.sb(st, "qab", [128, 512], F32)
            eexp = self.sb(st, "eexp", [64, 32, 128], BF16)
            cft = self.sb(st, "cft", [3, 6, 128], F32)
            bmaxt = self.sb(st, "bmaxt", [1, 400], F32)
            gat = self.sb(st, "gat", [128, nq, 36], F32)
            bq, bcst, bgat = Buf(), Buf(), Buf()
            P.dma("pool", lambda e: e.dma_start(out=qTs[:], in_=A["qT"][:, :, q0 * 128:(q0 + nq) * 128].rearrange(
                "h p (i q) -> p i h q", q=128)), writes=[bq])
            P.dma("pool", lambda e: e.dma_start(out=eexp[:], in_=A["eexp"]), writes=[bcst])
            P.dma("sp", lambda e: e.dma_start(out=cft[:], in_=A["cft"].rearrange("n c k -> c n k")), writes=[bcst])
            P.dma("sp", lambda e: e.dma_start(out=bmaxt[0:1, 0:384], in_=A["relb"]), writes=[bcst])
            P.dma("sp", lambda e: e.dma_start(out=gat[:], in_=A["gates"][q0 * 128:(q0 + nq) * 128, :].rearrange(
                "(i q) c -> q i c", q=128)), writes=[bgat])
            P.op("dve", lambda e: e.reduce_max(out=bmaxt[0:1, 384:385], in_=bmaxt[0:1, 0:384], axis=AX.X, apply_absolute_value=True),
                 reads=[bcst], writes=[bcst])
            for g in range(3):
                with ExitStack() as sg:
                    KT = [self.sb(sg, "KT%d" % i, [128, 4096], BF16) for i in range(2)]
                    VV = [self.sb(sg, "VV%d" % i, [128, 32, 129], BF16) for i in range(2)]
                    kam = self.sb(sg, "kam", [128, 2], F32)
                    nearT = self.sb(sg, "nearT", [128, 5, 512], F32)
                    rows3 = [self.sb(sg, "rows3%d" % i, [3, 512], F32) for i in range(2)]
                    bKV, bnear = Buf(), Buf()
                    brow = [Buf(), Buf()]
                    for br in range(2):
                        P.op("dve", lambda e, br=br: e.memset(VV[br][:, :, :], 1.0), writes=[bKV])
                        P.dma("pool", lambda e, br=br, g=g: e.dma_start(out=KT[br][:], in_=A["kT"][2 + br, g]), writes=[bKV])
                        col = (3 + 2 * br) * 384 + g * 128
                        P.dma("pool", lambda e, br=br, col=col: e.dma_start(
                            out=VV[br][:, :, 0:128], in_=A["kvtok"][0:4096, col:col + 128].rearrange("(kt p) c -> p kt c", p=128)),
                            writes=[bKV])
                        P.op("dve", lambda e, br=br: e.reduce_max(out=kam[:, br:br + 1], in_=KT[br][:, :], axis=AX.X,
                                                                  apply_absolute_value=True), reads=[bKV], writes=[bKV])
                        P.dma("sp", lambda e, br=br, g=g: e.dma_start(out=rows3[br][:], in_=A["rb31row"][g]), writes=[brow[br]])
                    P.dma("sp", lambda e, g=g: e.dma_start(out=nearT[:], in_=A["nearT"][g].rearrange("k p c -> p k c")), writes=[bnear])
                    for li, qi in enumerate(qis):
                        self._nsa_tile(sg, A, g, li, qi, qTs, bq, qab, eexp, cft, bmaxt, bcst, gat, bgat, KT, VV, kam, bKV,
                                       nearT, bnear, rows3, brow, kcmpT, vcmp, bkc)
                    P.fence()

    def _nsa_tile(self, sg, A, g, li, qi, qTs, bq, qab, eexp, cft, bmaxt, bcst, gat, bgat, KT, VV, kam, bKV,
                  nearT, bnear, rows3, brow, kcmpT, vcmp, bkc):
        P = self.P
        uq = 24 + qi
        with ExitStack() as st:
            cb = self.sb(st, "cb", [128, 256], F32)
            sc = self.sb(st, "sc", [128, 4, 64], F32)
            stt = self.sb(st, "stt", [128, 16], F32)
            imp = self.sb(st, "imp", [128, 64], F32)
            sbi = self.sb(st, "sbi", [128, 64], F32)
            m8 = self.sb(st, "m8", [128, 16], F32)
            sc2 = self.sb(st, "sc2", [128, 64], F32)
            sel = self.sb(st, "sel", [128, 64], F32)
            pcT = self.sb(st, "pcT", [64, 512], BF16)
            selT = self.sb(st, "selT", [64, 128], BF16)
            omix = self.sb(st, "omix", [128, 512], F32)
            EE = [self.sb(st, "EE%d" % i, [128, 512], F32) for i in range(2)]
            EB = [self.sb(st, "EB%d" % i, [128, 512], BF16) for i in range(2)]
            PT = [self.sb(st, "PT%d" % i, [128, 4, 128], BF16) for i in range(2)]
            gl = self.sb(st, "gl", [128, 8], F32)
            bcb, bsc, bstt, bimp, bsbi, bm8, bsc2, bsel, bpcT, bselT, bomix, bgl = [Buf() for _ in range(12)]
            bEE = [Buf(), Buf()]
            bEB = [Buf(), Buf()]
            bPT = [Buf(), Buf()]
            P.dma("sp", lambda e: e.dma_start(out=cb[:], in_=A["cmpbias"][qi][:, g * 256:(g + 1) * 256]), writes=[bcb])
            P.dma("sp", lambda e: e.dma_start(out=sbi[:], in_=A["selbias"][qi]), writes=[bsbi])
            P.op("pe", [lambda e, h=h: e.matmul(self.ps[0][:, h * 64:(h + 1) * 64], qTs[:, li, 4 * g + h, :],
                                                kcmpT[:, g * 64:(g + 1) * 64], start=True, stop=True) for h in range(4)],
                 reads=[bq, bkc], writes=[self.bps[0]])
            P.op("dve", lambda e: e.tensor_tensor(out=sc[:, :, :].rearrange("p h n -> p (h n)"), in0=self.ps[0][:, 0:256], in1=cb[:, :],
                                                  op=ALU.add), reads=[self.bps[0], bcb], writes=[bsc])
            P.op("dve", lambda e: e.reduce_max(out=stt[:, 0:4], in_=sc[:, :, :], axis=AX.X), reads=[bsc], writes=[bstt])
            P.op("dve", lambda e: e.tensor_scalar(stt[:, 0:4], stt[:, 0:4], -1000.0, -1.0, op0=ALU.max, op1=ALU.mult),
                 reads=[bstt], writes=[bstt])
            for h in range(4):
                P.op("act", lambda e, h=h: e.activation(out=sc[:, h, :], in_=sc[:, h, :], func=AF.Exp, bias=stt[:, h:h + 1]),
                     reads=[bsc, bstt], writes=[bsc])
            P.op("dve", lambda e: e.reduce_sum(out=stt[:, 4:8], in_=sc[:, :, :], axis=AX.X), reads=[bsc, bstt], writes=[bstt])
            P.op("dve", lambda e: e.tensor_scalar(stt[:, 4:8], stt[:, 4:8], 1e-30, None, op0=ALU.max), reads=[bstt], writes=[bstt])
            P.op("dve", lambda e: e.reciprocal(stt[:, 4:8], stt[:, 4:8]), reads=[bstt], writes=[bstt])
            for h in range(4):
                P.op("dve", lambda e, h=h: e.tensor_scalar(sc[:, h, :], sc[:, h, :], stt[:, 4 + h:5 + h], None, op0=ALU.mult),
                     reads=[bsc, bstt], writes=[bsc])
            P.op("dve", lambda e: e.tensor_tensor(out=imp[:, :], in0=sc[:, 0, :], in1=sc[:, 1, :], op=ALU.add), reads=[bsc], writes=[bimp])
            P.op("dve", lambda e: e.tensor_tensor(out=imp[:, :], in0=imp[:, :], in1=sc[:, 2, :], op=ALU.add), reads=[bsc, bimp], writes=[bimp])
            P.op("dve", lambda e: e.tensor_tensor(out=imp[:, :], in0=imp[:, :], in1=sc[:, 3, :], op=ALU.add), reads=[bsc, bimp], writes=[bimp])
            P.op("dve", lambda e: e.tensor_tensor(out=imp[:, :], in0=imp[:, :], in1=sbi[:, :], op=ALU.add), reads=[bsbi, bimp], writes=[bimp])
            P.op("dve", lambda e: e.max(out=m8[:, 0:8], in_=imp[:, :]), reads=[bimp], writes=[bm8])
            P.op("dve", lambda e: e.match_replace(out=sc2[:, :], in_to_replace=m8[:, 0:8], in_values=imp[:, :], imm_value=-1e9),
                 reads=[bimp, bm8], writes=[bsc2])
            P.op("dve", lambda e: e.max(out=m8[:, 8:16], in_=sc2[:, :]), reads=[bsc2, bm8], writes=[bm8])
            P.op("dve", lambda e: e.tensor_scalar(sel[:, :], imp[:, :], m8[:, 15:16], None, op0=ALU.is_ge), reads=[bimp, bm8], writes=[bsel])
            P.op("pe", [lambda e, h=h: e.transpose(self.ps[1][0:64, h * 128:(h + 1) * 128], sc[:, h, :], self.ident[:, :])
                        for h in range(4)], reads=[bsc, self.bconst], writes=[self.bps[1]])
            P.op("act", lambda e: e.activation(out=pcT[:, :], in_=self.ps[1][0:64, 0:512], func=AF.Copy), reads=[self.bps[1]], writes=[bpcT])
            P.op("pe", lambda e: e.transpose(self.ps[2][0:64, 0:128], sel[:, :], self.ident[:, :]), reads=[bsel, self.bconst],
                 writes=[self.bps[2]])
            P.op("act", lambda e: e.activation(out=selT[:, :], in_=self.ps[2][0:64, 0:128], func=AF.Copy), reads=[self.bps[2]], writes=[bselT])
            P.op("pe", [lambda e, h=h: e.matmul(self.ps[3][:, h * 128:(h + 1) * 128], pcT[:, h * 128:(h + 1) * 128], vcmp[:, g, :],
                                                start=True, stop=True) for h in range(4)], reads=[bpcT, bkc], writes=[self.bps[3]])
            for h in range(4):
                hd = 4 * g + h
                P.op("dve", lambda e, h=h, hd=hd: e.tensor_scalar(omix[:, h * 128:(h + 1) * 128], self.ps[3][:, h * 128:(h + 1) * 128],
                                                                  gat[:, li, 3 * hd:3 * hd + 1], None, op0=ALU.mult),
                     reads=[self.bps[3], bgat], writes=[bomix])
            P.op("act", lambda e: e.activation(out=qab[:, :], in_=qTs[:, li, 4 * g:4 * g + 4, :].rearrange("p h q -> p (h q)"),
                                               func=AF.Abs), reads=[bq], writes=[bcst])
            for br in range(2):
                if br == 0:
                    kts = list(range(0, uq + 1))
                else:
                    kts = list(range(uq - 4, uq + 1))
                P.op("pe", lambda e, br=br: e.matmul(self.ps[2][0:1, 0:512], kam[:, br:br + 1], qab[:, :], start=True, stop=True),
                     reads=[bKV, bcst], writes=[self.bps[2]])
                P.op("dve", lambda e, br=br: e.tensor_scalar(rows3[br][0:1, :], self.ps[2][0:1, 0:512], bmaxt[0:1, 384:385], -1.0,
                                                             op0=ALU.add, op1=ALU.mult),
                     reads=[self.bps[2], bcst], writes=[brow[br]])
                for ki, kt in enumerate(kts):
                    kk = uq - kt
                    near = kk <= (1 if br == 0 else 4)
                    if kt < 24:
                        pat = 3 if near else kt // 8
                    else:
                        pat = 5 if near else 4
                    sb_ = ki % 2
                    P.op("pe", [
                        lambda e, kt=kt, br=br, sb_=sb_: e.matmul(self.ps[sb_][:, 0:512], KT[br][:, kt * 128:(kt + 1) * 128],
                                                                  qTs[:, li, 4 * g:4 * g + 4, :], start=True, stop=False),
                        lambda e, pat=pat, br=br, sb_=sb_: e.matmul(self.ps[sb_][:, 0:512], cft[0:3, pat, :], rows3[br][0:3, :],
                                                                    start=False, stop=True)],
                        reads=[bKV, bq, bcst, brow[br]], writes=[self.bps[sb_]])
                    if near:
                        P.op("dve", lambda e, kk=kk, sb_=sb_: e.tensor_tensor(out=EE[sb_][:, :], in0=self.ps[sb_][:, 0:512],
                                                                              in1=nearT[:, kk, :], op=ALU.add),
                             reads=[self.bps[sb_], bnear], writes=[bEE[sb_]])
                        src, bsrc = EE[sb_], bEE[sb_]
                    else:
                        src, bsrc = self.ps[sb_], self.bps[sb_]
                    if br == 0:
                        P.op("act", lambda e, src=src, sb_=sb_: e.activation(out=EE[sb_][:, :], in_=src[:, 0:512], func=AF.Exp),
                             reads=[bsrc], writes=[bEE[sb_]])
                        P.op("pe", lambda e, kt=kt: e.matmul(self.ps[2][:, 0:128], eexp[:, kt, :], selT[:, :], start=True, stop=True),
                             reads=[bcst, bselT], writes=[self.bps[2]])
                        P.op("dve", [lambda e, h=h, sb_=sb_: e.tensor_tensor(out=PT[sb_][:, h, :], in0=EE[sb_][:, h * 128:(h + 1) * 128],
                                                                             in1=self.ps[2][:, 0:128], op=ALU.mult) for h in range(4)],
                             reads=[bEE[sb_], self.bps[2]], writes=[bPT[sb_]])
                        lhs = [PT[sb_][:, h, :] for h in range(4)]
                        blhs = bPT[sb_]
                    else:
                        P.op("act", lambda e, src=src, sb_=sb_: e.activation(out=EB[sb_][:, :], in_=src[:, 0:512], func=AF.Exp),
                             reads=[bsrc], writes=[bEB[sb_]])
                        lhs = [EB[sb_][:, h * 128:(h + 1) * 128] for h in range(4)]
                        blhs = bEB[sb_]
                    for h in range(4):
                        P.op("pe", lambda e, h=h, kt=kt, br=br, l=lhs[h], ki=ki, n=len(kts): e.matmul(
                            self.ps[4 + h][:, 0:129], l, VV[br][:, kt, :], start=(ki == 0), stop=(ki == n - 1)),
                            reads=[blhs, bKV], writes=[self.bps[4 + h]])
                for h in range(4):
                    hd = 4 * g + h
                    P.op("dve", lambda e, h=h: e.tensor_scalar(gl[:, h:h + 1], self.ps[4 + h][:, 128:129], 1e-30, None, op0=ALU.max),
                         reads=[self.bps[4 + h]], writes=[bgl])
                    P.op("dve", lambda e, h=h: e.reciprocal(gl[:, h:h + 1], gl[:, h:h + 1]), reads=[bgl], writes=[bgl])
                    P.op("dve", lambda e, h=h, hd=hd, br=br: e.tensor_tensor(out=gl[:, h:h + 1], in0=gl[:, h:h + 1],
                                                                             in1=gat[:, li, 3 * hd + 1 + br:3 * hd + 2 + br], op=ALU.mult),
                         reads=[bgl, bgat], writes=[bgl])
                    P.op("dve", lambda e, h=h: e.scalar_tensor_tensor(out=omix[:, h * 128:(h + 1) * 128], in0=self.ps[4 + h][:, 0:128],
                                                                      scalar=gl[:, h:h + 1], in1=omix[:, h * 128:(h + 1) * 128],
                                                                      op0=ALU.mult, op1=ALU.add),
                         reads=[self.bps[4 + h], bgl, bomix], writes=[bomix])
            P.dma("sp", lambda e: e.dma_start(out=A["tokmix"][qi * 128:(qi + 1) * 128, g * 512:(g + 1) * 512], in_=omix[:, :]),
                  reads=[bomix], writes=[self.bout])
            P.fence()

    def build_nsa_test(self):
        nc = self.nc
        P = self.P
        A = {"kT": self.din("kT", [4, 3, 128, 4096]), "kvtok": self.din("kvtok", [4096, 2304]),
             "qT": self.din("qT", [12, 128, 1024]), "gates": self.din("gates", [1024, 36]),
             "cmpbias": self.din("cmpbias", [8, 128, 768]), "selbias": self.din("selbias", [8, 128, 64]),
             "nearT": self.din("nearT", [3, 5, 128, 512]), "cft": self.din("cft", [6, 3, 128]),
             "rb31row": self.din("rb31row", [3, 3, 512]), "eexp": self.din("eexp", [64, 32, 128]),
             "relb": self.din("relb", [1, 384]), "w1": self.din("w1", [2, 64, 128, 256]), "w2": self.din("w2", [2, 256, 128]),
             "peT": self.din("peT", [2, 128, 64]), "b1c": self.din("b1c", [2, 128, 2]),
             "tokmix": self.dout("tokmix", [1024, 1536])}
        consts = self.din("consts", [128, 384])
        self.bout = Buf("out")
        with ExitStack() as st:
            P.alloc_sems(st)
            self.ps = [st.enter_context(nc.psum_tensor("ps%d" % i, [128, 512], F32)) for i in range(8)]
            self.bps = [Buf("ps%d" % i) for i in range(8)]
            cst = self.sb(st, "cst", [128, 384], F32)
            self.ones = cst[:, 0:128]
            self.utri = cst[:, 128:256]
            self.ident = cst[:, 256:384]
            self.bconst = Buf("const")
            P.dma("sp", lambda e: e.dma_start(out=cst[:], in_=consts), writes=[self.bconst])
            kcmpT, vcmp, bkc = self.nsa_compress(st, A)
            self.nsa_attn(A, list(range(self.stages.get("nq", 8))), kcmpT, vcmp, bkc)
            P.fence()
            block = st.enter_context(nc.Block())
            P.emit_all(block)
        return nc

    def nsa_mix_pass(self, N, off, mixT, bmixT):
        P = self.P
        with ExitStack() as st:
            qmT = self.sb(st, "qmT1", [128, 4, 512], BF16)
            mK = self.sb(st, "mK1", [128, 4, 256], BF16)
            mV = self.sb(st, "mV1", [128, 2, 512], BF16)
            mix = self.sb(st, "mix1", [128, 2048], F32)
            pp = self.sb(st, "pp1", [128, 256], F32)
            pT = self.sb(st, "pT1", [128, 2, 128], BF16)
            mst = self.sb(st, "mst1", [128, 8], F32)
            bqm, bmem, bmix, bpp, bpT, bmst = [Buf() for _ in range(6)]
            P.dma("pool", lambda e: e.dma_start(out=qmT[:], in_=N["qmT_d"][:, :, off:off + 512].rearrange("h p t -> p h t")),
                  reads=[self.bnsa], writes=[bqm])
            P.dma("pool", lambda e: e.dma_start(out=mK[:], in_=self.memK_d[1]), reads=[self.bmemd], writes=[bmem])
            P.dma("pool", lambda e: e.dma_start(out=mV[:], in_=self.memV_d[1]), reads=[self.bmemd], writes=[bmem])
            for ti in range(4):
                o0 = ti * 128
                nt = 128
                P.dma("sp", lambda e, o0=o0: e.dma_start(out=mix[:, 0:1536], in_=N["tokmix"][off + o0:off + o0 + 128, :]),
                      reads=[self.bout], writes=[bmix])
                for h in range(4):
                    pb = 4 + (h % 2) * 2
                    P.op("pe", lambda e, h=h, pb=pb, o0=o0: e.matmul(
                        self.ps[pb][0:nt, 0:256], qmT[:, h, o0:o0 + nt], mK[:, h, :], start=True, stop=True),
                        reads=[bqm, bmem], writes=[self.bps[pb]])
                    P.op("dve", lambda e, pb=pb: e.reduce_max(out=mst[0:nt, 0:1], in_=self.ps[pb][0:nt, 0:256], axis=AX.X),
                         reads=[self.bps[pb]], writes=[bmst])
                    P.op("dve", lambda e: e.tensor_scalar(mst[0:nt, 0:1], mst[0:nt, 0:1], -1.0, None, op0=ALU.mult),
                         reads=[bmst], writes=[bmst])
                    P.op("act", lambda e, pb=pb: e.activation(out=pp[0:nt, :], in_=self.ps[pb][0:nt, 0:256], func=AF.Exp,
                                                              bias=mst[0:nt, 0:1]), reads=[self.bps[pb], bmst], writes=[bpp])
                    P.op("dve", lambda e: e.reduce_sum(out=mst[0:nt, 1:2], in_=pp[0:nt, :], axis=AX.X), reads=[bpp, bmst], writes=[bmst])
                    P.op("dve", lambda e: e.reciprocal(mst[0:nt, 1:2], mst[0:nt, 1:2]), reads=[bmst], writes=[bmst])
                    for mt in range(2):
                        P.op("pe", lambda e, pb=pb, mt=mt: e.transpose(
                            self.ps[pb + 1][0:128, mt * 128:mt * 128 + nt], pp[0:nt, mt * 128:(mt + 1) * 128],
                            self.ident[0:nt, 0:nt]), reads=[bpp, self.bconst], writes=[self.bps[pb + 1]])
                    P.op("act", [lambda e, pb=pb, mt=mt: e.activation(
                        out=pT[:, mt, 0:nt], in_=self.ps[pb + 1][0:128, mt * 128:mt * 128 + nt],
                        func=AF.Copy) for mt in range(2)], reads=[self.bps[pb + 1]], writes=[bpT])
                    P.op("pe", [lambda e, pb=pb, mt=mt, h=h: e.matmul(
                        self.ps[pb][0:nt, 256:384], pT[:, mt, 0:nt], mV[:, mt, h * 128:(h + 1) * 128],
                        start=(mt == 0), stop=(mt == 1)) for mt in range(2)], reads=[bpT, bmem], writes=[self.bps[pb]])
                    P.op("dve", lambda e, pb=pb, h=h: e.tensor_scalar(
                        mix[0:nt, 1536 + h * 128:1536 + (h + 1) * 128], self.ps[pb][0:nt, 256:384], mst[0:nt, 1:2], None,
                        op0=ALU.mult), reads=[self.bps[pb], bmst], writes=[bmix])
                for fc in range(KC):
                    pb = (fc // 4) % 4
                    cc = (fc % 4) * 128
                    P.op("pe", lambda e, fc=fc, pb=pb, cc=cc: e.transpose(
                        self.ps[pb][0:128, cc:cc + nt], mix[0:nt, fc * 128:(fc + 1) * 128], self.ident[0:nt, 0:nt]),
                        reads=[bmix, self.bconst], writes=[self.bps[pb]])
                    if fc % 2 == 0:
                        P.op("act", lambda e, fc=fc, pb=pb, cc=cc, o0=o0: e.activation(
                            out=mixT[:, fc, o0:o0 + nt], in_=self.ps[pb][0:128, cc:cc + nt], func=AF.Copy),
                            reads=[self.bps[pb]], writes=[bmixT])
                    else:
                        P.op("dve", lambda e, fc=fc, pb=pb, cc=cc, o0=o0: e.tensor_copy(
                            out=mixT[:, fc, o0:o0 + nt], in_=self.ps[pb][0:128, cc:cc + nt]),
                            reads=[self.bps[pb]], writes=[bmixT])
            P.fence()

    def smp_pregather(self, Sm):
        P = self.P
        self.bscr = Buf("scr")
        with ExitStack() as st:
            ptf = self.sb(st, "g_ptf", [128, 128], F32)
            iop = self.sb(st, "g_iop", [128, 1], F32)
            idx = self.sb(st, "g_idx", [128, 128], I32)
            pg = [self.sb(st, "g_pg%d" % i, [128, 768], F32) for i in range(4)]
            bpg = [Buf() for _ in range(4)]
            bidx = Buf()
            def first(e):
                self._breg = e.to_reg(163839)
                return e.dma_start(out=ptf[:], in_=Sm["ptrep"])
            P.dma("pool", first, writes=[bidx])
            P.dma("sp", lambda e: e.dma_start(out=iop[:], in_=Sm["iotap"]), writes=[bidx])
            P.op("dve", lambda e: e.tensor_scalar(ptf[:, :], ptf[:, :], 128.0, iop[:, 0:1], op0=ALU.mult, op1=ALU.add),
                 reads=[bidx], writes=[bidx])
            P.op("dve", lambda e: e.tensor_copy(out=idx[:, :], in_=ptf[:, :]), reads=[bidx], writes=[bidx])
            k = 0
            for pool, scr in ((Sm["pool_cmp"], Sm["scr_cmp"]), (Sm["pool_slc"], Sm["scr_slc"])):
                for i in range(128):
                    gi = k % 4
                    k += 1
                    P.dma("pool", lambda e, i=i, gi=gi, pool=pool: e.indirect_dma_start(
                        out=pg[gi][:, :], out_offset=None, in_=pool[:, :],
                        in_offset=bass.IndirectOffsetOnAxis(ap=idx[:, i:i + 1], axis=0),
                        bounds_check=self._breg, oob_is_err=False), reads=[bidx], writes=[bpg[gi]])
                    P.dma("sp", lambda e, i=i, gi=gi, scr=scr: e.dma_start(out=scr[i * 128:(i + 1) * 128, :], in_=pg[gi][:, :]),
                          reads=[bpg[gi]], writes=[self.bscr])
            P.fence()

    def smp_nsa(self, Sm):
        P = self.P
        nc = self.nc
        with ExitStack() as st0:
            qTs = self.sb(st0, "s_qT", [128, 12], BF16)
            qTf = self.sb(st0, "s_qTf", [128, 12], F32)
            qab = self.sb(st0, "s_qab", [128, 12], F32)
            gT = self.sb(st0, "s_gT", [4, 9], F32)
            kcm = self.sb(st0, "s_kcm", [128, 3, 256], BF16)
            vcm = self.sb(st0, "s_vcm", [128, 2, 3, 128], BF16)
            selT = self.sb(st0, "s_selT", [128, 2, 3], BF16)
            omix = self.sb(st0, "s_omix", [4, 3, 128], F32)
            bmx = self.sb(st0, "s_bmx", [1, 400], F32)
            cfs = self.sb(st0, "s_cfs", [3, 2, 128], F32)
            knw = self.sb(st0, "s_knw", [128, 2, 3], F32)
            knb = self.sb(st0, "s_knb", [128, 2, 3], BF16)
            vnw = self.sb(st0, "s_vnw", [1, 2, 3, 129], BF16)
            bq, bg, bidx, bkc, bsel, bom, bc, bkn = [Buf() for _ in range(8)]
            P.dma("sp", lambda e: e.dma_start(out=qTf[:], in_=Sm["qTs_d"]), reads=[self.bnsa], writes=[bq])
            P.op("dve", lambda e: e.tensor_copy(out=qTs[:, :], in_=qTf[:, :]), reads=[bq], writes=[bq])
            P.op("act", lambda e: e.activation(out=qab[:, :], in_=qTf[:, :], func=AF.Abs), reads=[bq], writes=[bq])
            P.dma("sp", lambda e: e.dma_start(out=gT[:], in_=Sm["gT_d"]), reads=[self.bnsa], writes=[bg])
            P.dma("sp", lambda e: e.dma_start(out=bmx[0:1, 0:384], in_=Sm["relb"]), writes=[bc])
            P.op("dve", lambda e: e.reduce_max(out=bmx[0:1, 384:385], in_=bmx[0:1, 0:384], axis=AX.X, apply_absolute_value=True),
                 reads=[bc], writes=[bc])
            P.dma("sp", lambda e: e.dma_start(out=cfs[:], in_=Sm["cfs"].rearrange("n c k -> c n k")), writes=[bc])
            P.dma("sp", lambda e: e.dma_start(out=knw[:], in_=Sm["knew_d"]), reads=[self.bnsa], writes=[bkn])
            P.op("dve", lambda e: e.tensor_copy(out=knb[:, :, :], in_=knw[:, :, :]), reads=[bkn], writes=[bkn])
            P.op("dve", lambda e: e.memset(vnw[:, :, :, :], 1.0), writes=[bkn])
            if "vnew_d" in Sm:
                P.dma("pool", lambda e: e.dma_start(out=vnw[0:1, :, :, 0:128], in_=Sm["vnew_d"]), reads=[self.bnsa], writes=[bkn])
            else:
                P.dma("pool", lambda e: e.dma_start(out=vnw[0:1, 0, :, 0:128], in_=Sm["vnew_slc"].rearrange("o (g d) -> o g d", g=3)),
                      reads=[self.bout], writes=[bkn])
                P.dma("pool", lambda e: e.dma_start(out=vnw[0:1, 1, :, 0:128], in_=Sm["vnew_win"].rearrange("o (g d) -> o g d", g=3)),
                      reads=[self.bout], writes=[bkn])
            def gather(pool, i, dst, bdst):
                scr = Sm["scr_cmp"] if pool is Sm["pool_cmp"] else Sm["scr_slc"]
                P.dma("sp", lambda e, i=i, dst=dst, scr=scr: e.dma_start(out=dst[:, :], in_=scr[i * 128:(i + 1) * 128, :]),
                      reads=[self.bscr], writes=[bdst])

            def compress_pass(kv):
                with ExitStack() as st:
                    pg = [self.sb(st, "s_pg%d" % i, [128, 768], F32) for i in range(3)]
                    w1s = self.sb(st, "s_w1", [128, 64, 256], BF16)
                    w2s = self.sb(st, "s_w2", [128, 2, 128], BF16)
                    peT = self.sb(st, "s_pe", [128, 64], BF16)
                    b1c = self.sb(st, "s_b1", [128, 2], F32)
                    hbc = self.sb(st, "s_hb", [128, 2], F32)
                    cT = self.sb(st, "s_cT", [128, 3, 8192], BF16)
                    hidT = self.sb(st, "s_hid", [128, 2, 384], BF16)
                    bpg = [Buf() for _ in range(3)]
                    bw, bcT, bhb, bhid = Buf(), Buf(), Buf(), Buf()
                    P.dma("pool", lambda e, kv=kv: e.dma_start(out=w1s[:], in_=Sm["w1"][kv].rearrange("r p h -> p r h")), writes=[bw])
                    P.dma("pool", lambda e, kv=kv: e.dma_start(out=w2s[:], in_=Sm["w2"][kv].rearrange("(t p) d -> p t d", p=128)), writes=[bw])
                    P.dma("pool", lambda e, kv=kv: e.dma_start(out=peT[:], in_=Sm["peT"][kv]), writes=[bw])
                    P.dma("sp", lambda e, kv=kv: e.dma_start(out=b1c[:], in_=Sm["b1c"][kv]), writes=[bw])
                    for t in range(2):
                        P.op("pe", [lambda e, r=r, t=t: e.matmul(self.ps[3][:, t:t + 1], w1s[:, r, t * 128:(t + 1) * 128], peT[:, r:r + 1],
                                                                 start=(r == 0), stop=(r == 63)) for r in range(64)],
                             reads=[bw], writes=[self.bps[3]])
                    P.op("dve", lambda e: e.tensor_tensor(out=hbc[:, :], in0=self.ps[3][:, 0:2], in1=b1c[:, :], op=ALU.add),
                         reads=[self.bps[3], bw], writes=[bhb])
                    for ch in range(2):
                        for pl in range(64):
                            i = ch * 64 + pl
                            gi = i % 3
                            gather(Sm["pool_cmp"], i, pg[gi], bpg[gi])
                            pb = 4 + (i % 2)
                            P.op("pe", [lambda e, g=g, gi=gi, pb=pb, kv=kv: e.transpose(
                                self.ps[pb][:, g * 128:(g + 1) * 128], pg[gi][:, kv * 384 + g * 128:kv * 384 + (g + 1) * 128],
                                self.ident[:, :]) for g in range(3)], reads=[bpg[gi], self.bconst], writes=[self.bps[pb]])
                            src = self.ps[pb][:, 0:384].rearrange("p (g t) -> p g t", g=3)
                            if i % 2 == 0:
                                P.op("act", lambda e, src=src, pl=pl: e.activation(out=cT[:, :, pl * 128:(pl + 1) * 128], in_=src,
                                                                                   func=AF.Copy), reads=[self.bps[pb]], writes=[bcT])
                            else:
                                P.op("dve", lambda e, src=src, pl=pl: e.tensor_copy(out=cT[:, :, pl * 128:(pl + 1) * 128], in_=src),
                                     reads=[self.bps[pb]], writes=[bcT])
                        if "dbg_cT" in Sm and kv == 0 and ch == 0:
                            P.dma("sp", lambda e: e.dma_start(out=Sm["dbg_cT"], in_=cT[:, :, 0:256]), reads=[bcT], writes=[self.bout])
                            P.dma("sp", lambda e: e.dma_start(out=Sm["dbg_pg"], in_=pg[0][:, :]), reads=[bpg[0]], writes=[self.bout])
                            P.dma("sp", lambda e: e.dma_start(out=Sm["dbg_w1"], in_=w1s[:, 0:2, :]), reads=[bw], writes=[self.bout])
                        for t in range(2):
                            P.op("pe", [lambda e, r=r, t=t: e.matmul(self.ps[t][:, 0:384], w1s[:, r, t * 128:(t + 1) * 128],
                                                                     cT[:, :, r:8192:64], start=(r == 0), stop=(r == 63))
                                        for r in range(64)], reads=[bw, bcT], writes=[self.bps[t]])
                            P.op("act", lambda e, t=t: e.activation(out=hidT[:, t, :], in_=self.ps[t][:, 0:384], func=AF.Silu,
                                                                    bias=hbc[:, t:t + 1]), reads=[self.bps[t], bhb], writes=[bhid])
                        if kv == 0:
                            P.op("pe", [lambda e, t=t: e.matmul(self.ps[2][:, 0:384], w2s[:, t, :], hidT[:, t, :], start=(t == 0), stop=(t == 1))
                                        for t in range(2)], reads=[bw, bhid], writes=[self.bps[2]])
                            P.op("act", lambda e, ch=ch: e.activation(out=kcm[:, :, ch * 128:(ch + 1) * 128],
                                                                      in_=self.ps[2][:, 0:384].rearrange("p (g n) -> p g n", g=3),
                                                                      func=AF.Copy), reads=[self.bps[2]], writes=[bkc])
                        else:
                            for g in range(3):
                                P.op("pe", [lambda e, t=t, g=g: e.matmul(self.ps[2][:, g * 128:(g + 1) * 128], hidT[:, t, g * 128:(g + 1) * 128],
                                                                         w2s[:, t, :], start=(t == 0), stop=(t == 1)) for t in range(2)],
                                     reads=[bw, bhid], writes=[self.bps[2]])
                            P.op("act", lambda e, ch=ch: e.activation(out=vcm[:, ch, :, :],
                                                                      in_=self.ps[2][:, 0:384].rearrange("p (g d) -> p g d", g=3),
                                                                      func=AF.Copy), reads=[self.bps[2]], writes=[bkc])
                    P.fence()
            for kv_ in range(2):
                compress_pass(kv_)
            if "dbg_kcm" in Sm:
                P.dma("sp", lambda e: e.dma_start(out=Sm["dbg_kcm"], in_=kcm[:, :, :]), reads=[bkc], writes=[self.bout])
                P.dma("sp", lambda e: e.dma_start(out=Sm["dbg_vcm"], in_=vcm[:, :, :, :]), reads=[bkc], writes=[self.bout])
            with ExitStack() as st:
                cb = self.sb(st, "s_cb", [4, 256], F32)
                sc = self.sb(st, "s_sc", [4, 256], F32)
                stt = self.sb(st, "s_st", [4, 8], F32)
                imp = self.sb(st, "s_imp", [1, 256], F32)
                sbi = self.sb(st, "s_sbi", [1, 256], F32)
                m8 = self.sb(st, "s_m8", [1, 16], F32)
                sc2 = self.sb(st, "s_sc2", [1, 256], F32)
                sel = self.sb(st, "s_sel", [1, 256], F32)
                pcT = self.sb(st, "s_pcT", [128, 2, 4], BF16)
                bcb, bsc, bstt, bimp, bsbi, bm8, bsc2, bsl, bpc = [Buf() for _ in range(9)]
                P.dma("sp", lambda e: e.dma_start(out=sbi[:], in_=Sm["selb_s"]), writes=[bsbi])
                for g in range(3):
                    P.dma("sp", lambda e, g=g: e.dma_start(out=cb[:], in_=Sm["cmpb_s"][g]), writes=[bcb])
                    P.op("pe", lambda e, g=g: e.matmul(self.ps[0][0:4, 0:256], qTs[:, 4 * g:4 * g + 4], kcm[:, g, :], start=True, stop=True),
                         reads=[bq, bkc], writes=[self.bps[0]])
                    P.op("dve", lambda e: e.tensor_tensor(out=sc[:, :], in0=self.ps[0][0:4, 0:256], in1=cb[:, :], op=ALU.add),
                         reads=[self.bps[0], bcb], writes=[bsc])
                    P.op("dve", lambda e: e.reduce_max(out=stt[:, 0:1], in_=sc[:, :], axis=AX.X), reads=[bsc], writes=[bstt])
                    P.op("dve", lambda e: e.tensor_scalar(stt[:, 0:1], stt[:, 0:1], -1.0, None, op0=ALU.mult), reads=[bstt], writes=[bstt])
                    P.op("act", lambda e: e.activation(out=sc[:, :], in_=sc[:, :], func=AF.Exp, bias=stt[:, 0:1]),
                         reads=[bsc, bstt], writes=[bsc])
                    P.op("dve", lambda e: e.reduce_sum(out=stt[:, 1:2], in_=sc[:, :], axis=AX.X), reads=[bsc, bstt], writes=[bstt])
                    P.op("dve", lambda e: e.reciprocal(stt[:, 1:2], stt[:, 1:2]), reads=[bstt], writes=[bstt])
                    P.op("dve", lambda e: e.tensor_scalar(sc[:, :], sc[:, :], stt[:, 1:2], None, op0=ALU.mult), reads=[bsc, bstt], writes=[bsc])
                    P.op("pe", lambda e: e.matmul(self.ps[1][0:1, 0:256], self.ones[0:4, 0:1], sc[:, :], start=True, stop=True),
                         reads=[bsc, self.bconst], writes=[self.bps[1]])
                    P.op("dve", lambda e: e.tensor_tensor(out=imp[:, :], in0=self.ps[1][0:1, 0:256], in1=sbi[:, :], op=ALU.add),
                         reads=[self.bps[1], bsbi], writes=[bimp])
                    P.op("dve", lambda e: e.max(out=m8[:, 0:8], in_=imp[:, :]), reads=[bimp], writes=[bm8])
                    P.op("dve", lambda e: e.match_replace(out=sc2[:, :], in_to_replace=m8[:, 0:8], in_values=imp[:, :], imm_value=-1e9),
                         reads=[bimp, bm8], writes=[bsc2])
                    P.op("dve", lambda e: e.max(out=m8[:, 8:16], in_=sc2[:, :]), reads=[bsc2, bm8], writes=[bm8])
                    P.op("dve", lambda e: e.tensor_scalar(sel[:, :], imp[:, :], m8[:, 14:15], None, op0=ALU.is_ge), reads=[bimp, bm8], writes=[bsl])
                    for t in range(2):
                        P.op("pe", lambda e, t=t: e.transpose(self.ps[2][:, t:t + 1], sel[0:1, t * 128:(t + 1) * 128], self.ident[0:1, 0:1]),
                             reads=[bsl, self.bconst], writes=[self.bps[2]])
                        P.op("pe", lambda e, t=t: e.transpose(self.ps[3][:, t * 4:t * 4 + 4], sc[0:4, t * 128:(t + 1) * 128], self.ident[0:4, 0:4]),
                             reads=[bsc, self.bconst], writes=[self.bps[3]])
                    P.op("act", lambda e, g=g: e.activation(out=selT[:, :, g], in_=self.ps[2][:, 0:2], func=AF.Copy),
                         reads=[self.bps[2]], writes=[bsel])
                    P.op("act", lambda e: e.activation(out=pcT[:, :, :], in_=self.ps[3][:, 0:8].rearrange("p (t h) -> p t h", t=2),
                                                       func=AF.Copy), reads=[self.bps[3]], writes=[bpc])
                    P.op("pe", [lambda e, t=t, g=g: e.matmul(self.ps[4][0:4, 0:128], pcT[:, t, :], vcm[:, t, g, :], start=(t == 0), stop=(t == 1))
                                for t in range(2)], reads=[bpc, bkc], writes=[self.bps[4]])
                    P.op("dve", lambda e, g=g: e.tensor_scalar(omix[:, g, :], self.ps[4][0:4, 0:128], gT[:, g:g + 1], None, op0=ALU.mult),
                         reads=[self.bps[4], bg], writes=[bom])
                P.fence()
            with ExitStack() as st:
                eex = self.sb(st, "s_eex", [128, 64, 128], BF16)
                kam = self.sb(st, "s_kam", [128, 2, 3], F32)
                kat = self.sb(st, "s_kat", [128, 3], F32)
                rows3 = self.sb(st, "s_rows", [3, 2, 3, 4], F32)
                nrs = self.sb(st, "s_nrs", [128, 3, 4], F32)
                nrw = self.sb(st, "s_nrw", [128, 3, 4, 4], F32)
                b0 = self.sb(st, "s_b0", [1, 3, 4], F32)
                KT = self.sb(st, "s_KT", [128, 3, 128], BF16)
                KW = self.sb(st, "s_KW", [128, 3, 512], BF16)
                VS = self.sb(st, "s_VS", [128, 3, 129], BF16)
                VW = self.sb(st, "s_VW", [128, 4, 3, 129], BF16)
                EE = self.sb(st, "s_EE", [128, 3, 4], F32)
                PTt = self.sb(st, "s_PT", [128, 3, 4], BF16)
                mcol = self.sb(st, "s_mc", [128, 3], F32)
                gl = self.sb(st, "s_gl", [4, 4], F32)
                pq = [self.sb(st, "s_pgs%d" % i, [128, 768], F32) for i in range(3)]
                bpq = [Buf() for _ in range(3)]
                bt, bkam, brow, bKT, bKW, bVS, bVW, bEE, bPT, bmc, bgl = [Buf() for _ in range(11)]
                P.dma("pool", lambda e: e.dma_start(out=eex[:], in_=Sm["eexp_s"]), writes=[bt])
                P.dma("sp", lambda e: e.dma_start(out=nrs[:], in_=Sm["nears_s"].rearrange("g k h -> k g h")), writes=[bt])
                P.dma("sp", lambda e: e.dma_start(out=nrw[:], in_=Sm["nearw_s"].rearrange("g t k h -> k g t h")), writes=[bt])
                P.dma("sp", lambda e: e.dma_start(out=b0[:], in_=Sm["b0_s"]), writes=[bt])
                for br in range(2):
                    P.dma("sp", lambda e, br=br: e.dma_start(out=rows3[:, br, :, :], in_=Sm["rb31_s"].rearrange("g r h -> r g h")), writes=[brow])
                P.op("dve", lambda e: e.memset(VS[:, :, :], 1.0), writes=[bVS])
                P.op("dve", lambda e: e.memset(VW[:, :, :, :], 1.0), writes=[bVW])
                P.op("act", lambda e: e.activation(out=kam[:, :, :], in_=knw[:, :, :], func=AF.Abs), reads=[bkn], writes=[bkam])
                for t in range(4):
                    gi = t % 3
                    P.dma("sp", lambda e, t=t, gi=gi: e.dma_start(out=pq[gi][:], in_=Sm["swin"][t * 128:(t + 1) * 128, :]), writes=[bpq[gi]])
                    pb = 4 + (t % 2)
                    P.op("pe", [lambda e, g=g, gi=gi, pb=pb: e.transpose(self.ps[pb][:, g * 128:(g + 1) * 128], pq[gi][:, g * 128:(g + 1) * 128],
                                                                         self.ident[:, :]) for g in range(3)],
                         reads=[bpq[gi], self.bconst], writes=[self.bps[pb]])
                    P.op("act", lambda e, pb=pb, t=t: e.activation(out=KW[:, :, t * 128:(t + 1) * 128],
                                                                   in_=self.ps[pb][:, 0:384].rearrange("p (g t) -> p g t", g=3), func=AF.Copy),
                         reads=[self.bps[pb]], writes=[bKW])
                    P.op("dve", lambda e, gi=gi, t=t: e.tensor_copy(out=VW[:, t, :, 0:128],
                                                                    in_=pq[gi][:, 384:768].rearrange("p (g d) -> p g d", g=3)),
                         reads=[bpq[gi]], writes=[bVW])
                P.op("dve", lambda e: e.reduce_max(out=kat[:, :], in_=KW[:, :, :], axis=AX.X, apply_absolute_value=True), reads=[bKW], writes=[bkam])
                P.op("dve", lambda e: e.tensor_tensor(out=kam[:, 1, :], in0=kam[:, 1, :], in1=kat[:, :], op=ALU.max), reads=[bkam], writes=[bkam])
                for i in range(128):
                    gi = i % 3
                    gather(Sm["pool_slc"], i, pq[gi], bpq[gi])
                    pb = 4 + (i % 2)
                    P.op("pe", [lambda e, g=g, gi=gi, pb=pb: e.transpose(self.ps[pb][:, g * 128:(g + 1) * 128], pq[gi][:, g * 128:(g + 1) * 128],
                                                                         self.ident[:, :]) for g in range(3)],
                         reads=[bpq[gi], self.bconst], writes=[self.bps[pb]])
                    P.op("dve", lambda e, pb=pb: e.reduce_max(out=kat[:, :], in_=self.ps[pb][:, 0:384].rearrange("p (g t) -> p g t", g=3),
                                                              axis=AX.X, apply_absolute_value=True), reads=[self.bps[pb], bkam], writes=[bkam])
                    P.op("dve", lambda e: e.tensor_tensor(out=kam[:, 0, :], in0=kam[:, 0, :], in1=kat[:, :], op=ALU.max), reads=[bkam], writes=[bkam])
                for br in range(2):
                    for g in range(3):
                        P.op("pe", lambda e, br=br, g=g: e.matmul(self.ps[0][0:1, 0:4], kam[:, br, g:g + 1], qab[:, 4 * g:4 * g + 4],
                                                                  start=True, stop=True), reads=[bkam, bq], writes=[self.bps[0]])
                        P.op("dve", lambda e, br=br, g=g: e.tensor_scalar(rows3[0:1, br, g, :], self.ps[0][0:1, 0:4], bmx[0:1, 384:385], -1.0,
                                                                          op0=ALU.add, op1=ALU.mult), reads=[self.bps[0], bc], writes=[brow])

                def key_tile(br, g, nk, lhsK, pat, extra, extra_b, first, last, Vrhs, bV, mask):
                    pb = 1 + (g % 2)
                    P.op("pe", [lambda e: e.matmul(self.ps[pb][0:nk, 0:4], lhsK, qTs[:, 4 * g:4 * g + 4], start=True, stop=False),
                                lambda e: e.matmul(self.ps[pb][0:nk, 0:4], cfs[0:3, pat, 0:nk], rows3[0:3, br, g, :], start=False, stop=True)],
                         reads=[bKT, bKW, bkn, bq, bc, brow], writes=[self.bps[pb]])
                    if extra is not None:
                        P.op("dve", lambda e: e.tensor_tensor(out=EE[0:nk, g, :], in0=self.ps[pb][0:nk, 0:4], in1=extra, op=ALU.add),
                             reads=[self.bps[pb], extra_b], writes=[bEE])
                        P.op("act", lambda e: e.activation(out=EE[0:nk, g, :], in_=EE[0:nk, g, :], func=AF.Exp), reads=[bEE], writes=[bEE])
                    else:
                        P.op("act", lambda e: e.activation(out=EE[0:nk, g, :], in_=self.ps[pb][0:nk, 0:4], func=AF.Exp),
                             reads=[self.bps[pb]], writes=[bEE])
                    if mask:
                        P.op("dve", lambda e: e.tensor_scalar(PTt[0:nk, g, :], EE[0:nk, g, :], mcol[0:nk, g:g + 1], None, op0=ALU.mult),
                             reads=[bEE, bmc], writes=[bPT])
                    else:
                        P.op("dve", lambda e: e.tensor_copy(out=PTt[0:nk, g, :], in_=EE[0:nk, g, :]), reads=[bEE], writes=[bPT])
                    P.op("pe", lambda e: e.matmul(self.ps[5 + g][0:4, 0:129], PTt[0:nk, g, :], Vrhs, start=first, stop=last),
                         reads=[bPT, bV], writes=[self.bps[5 + g]])

                def finalize(br):
                    for g in range(3):
                        P.op("dve", lambda e, g=g: e.tensor_scalar(gl[:, 0:1], self.ps[5 + g][0:4, 128:129], 1e-30, None, op0=ALU.max),
                             reads=[self.bps[5 + g]], writes=[bgl])
                        P.op("dve", lambda e: e.reciprocal(gl[:, 0:1], gl[:, 0:1]), reads=[bgl], writes=[bgl])
                        P.op("dve", lambda e, g=g: e.tensor_tensor(out=gl[:, 0:1], in0=gl[:, 0:1], in1=gT[:, 3 * (1 + br) + g:3 * (1 + br) + g + 1],
                                                                   op=ALU.mult), reads=[bgl, bg], writes=[bgl])
                        P.op("dve", lambda e, g=g: e.scalar_tensor_tensor(out=omix[:, g, :], in0=self.ps[5 + g][0:4, 0:128], scalar=gl[:, 0:1],
                                                                          in1=omix[:, g, :], op0=ALU.mult, op1=ALU.add),
                             reads=[self.bps[5 + g], bgl, bom], writes=[bom])

                for g in range(3):
                    for t in range(4):
                        key_tile(1, g, 128, KW[:, g, t * 128:(t + 1) * 128], 1, nrw[:, g, t, :], bt, t == 0, False, VW[:, t, g, :], bVW, False)
                    key_tile(1, g, 1, knb[:, 1, g:g + 1], 1, b0[0:1, g, :], bt, False, True, vnw[0:1, 1, g, :], bkn, False)
                finalize(1)
                for i in range(128):
                    gi = i % 3
                    gather(Sm["pool_slc"], i, pq[gi], bpq[gi])
                    pb = 4 if i % 2 == 0 else 0
                    P.op("pe", [lambda e, g=g, gi=gi, pb=pb: e.transpose(self.ps[pb][:, g * 128:(g + 1) * 128], pq[gi][:, g * 128:(g + 1) * 128],
                                                                         self.ident[:, :]) for g in range(3)],
                         reads=[bpq[gi], self.bconst], writes=[self.bps[pb]])
                    P.op("act", lambda e, pb=pb: e.activation(out=KT[:, :, :], in_=self.ps[pb][:, 0:384].rearrange("p (g t) -> p g t", g=3),
                                                              func=AF.Copy), reads=[self.bps[pb]], writes=[bKT])
                    P.op("dve", lambda e, gi=gi: e.tensor_copy(out=VS[:, :, 0:128], in_=pq[gi][:, 384:768].rearrange("p (g d) -> p g d", g=3)),
                         reads=[bpq[gi]], writes=[bVS])
                    P.op("pe", lambda e, i=i: e.matmul(self.ps[3][:, 0:3], eex[:, i % 64, :], selT[:, i // 64, :], start=True, stop=True),
                         reads=[bt, bsel], writes=[self.bps[3]])
                    P.op("act", lambda e: e.activation(out=mcol[:, :], in_=self.ps[3][:, 0:3], func=AF.Copy), reads=[self.bps[3]], writes=[bmc])
                    for g in range(3):
                        if i == 127:
                            key_tile(0, g, 128, KT[:, g, :], 1, nrs[:, g, :], bt, False, False, VS[:, g, :], bVS, True)
                        else:
                            key_tile(0, g, 128, KT[:, g, :], 0, None, None, i == 0, False, VS[:, g, :], bVS, True)
                for g in range(3):
                    key_tile(0, g, 1, knb[:, 0, g:g + 1], 1, b0[0:1, g, :], bt, False, True, vnw[0:1, 0, g, :], bkn, False)
                finalize(0)
                oT = self.sb(st, "s_oT", [128, 12], F32)
                boT = Buf()
                for g in range(3):
                    P.op("pe", lambda e, g=g: e.transpose(self.ps[0][:, g * 4:g * 4 + 4], omix[0:4, g, :], self.ident[0:4, 0:4]),
                         reads=[bom, self.bconst], writes=[self.bps[0]])
                P.op("act", lambda e: e.activation(out=oT[:, :], in_=self.ps[0][:, 0:12], func=AF.Copy), reads=[self.bps[0]], writes=[boT])
                P.dma("sp", lambda e: e.dma_start(out=Sm["mixTs"], in_=oT[:, :]), reads=[boT], writes=[self.bout])
                P.fence()

    def build_smp_test(self):
        nc = self.nc
        P = self.P
        Sm = {k: self.din(k, shp, dt) for k, shp, dt in [
            ("pool_cmp", [163840, 768], F32), ("pool_slc", [163840, 768], F32), ("ptrep", [128, 128], I32), ("iotap", [128, 1], F32),
            ("swin", [512, 768], F32), ("qTs_d", [128, 12], F32), ("gT_d", [4, 9], F32), ("knew_d", [128, 2, 3], F32),
            ("vnew_d", [1, 2, 3, 128], F32), ("relb", [1, 384], F32), ("cfs", [2, 3, 128], F32), ("w1", [2, 64, 128, 256], F32),
            ("w2", [2, 256, 128], F32), ("peT", [2, 128, 64], F32), ("b1c", [2, 128, 2], F32), ("selb_s", [1, 256], F32),
            ("cmpb_s", [3, 4, 256], F32), ("eexp_s", [128, 64, 128], F32), ("nears_s", [3, 128, 4], F32),
            ("nearw_s", [3, 4, 128, 4], F32), ("b0_s", [1, 3, 4], F32), ("rb31_s", [3, 3, 4], F32)]}
        Sm["scr_cmp"] = nc.dram_tensor("scr_cmp", [16384, 768], F32).ap()
        Sm["scr_slc"] = nc.dram_tensor("scr_slc", [16384, 768], F32).ap()
        Sm["mixTs"] = self.dout("mixTs", [128, 12])
        Sm["dbg_kcm"] = self.dout("dbg_kcm", [128, 3, 256], BF16)
        Sm["dbg_cT"] = self.dout("dbg_cT", [128, 3, 256], BF16)
        Sm["dbg_pg"] = self.dout("dbg_pg", [128, 768], F32)
        Sm["dbg_pg0"] = self.dout("dbg_pg0", [128, 768], F32)
        Sm["dbg_w1"] = self.dout("dbg_w1", [128, 2, 256], BF16)
        Sm["dbg_vcm"] = self.dout("dbg_vcm", [128, 2, 3, 128], BF16)
        consts = self.din("consts", [128, 384])
        self.bout, self.bnsa = Buf("out"), Buf("nsa")
        with ExitStack() as st:
            P.alloc_sems(st)
            self.ps = [st.enter_context(nc.psum_tensor("ps%d" % i, [128, 512], F32)) for i in range(8)]
            self.bps = [Buf("ps%d" % i) for i in range(8)]
            cst = self.sb(st, "cst", [128, 384], F32)
            self.ones = cst[:, 0:128]
            self.utri = cst[:, 128:256]
            self.ident = cst[:, 256:384]
            self.bconst = Buf("const")
            P.dma("sp", lambda e: e.dma_start(out=cst[:], in_=consts), writes=[self.bconst])
            self.smp_pregather(Sm)
            if "dbg_pg0" in Sm:
                with ExitStack() as std:
                    td = self.sb(std, "dbgt", [128, 768], F32)
                    btd = Buf()
                    P.dma("sp", lambda e: e.dma_start(out=td[:], in_=Sm["scr_cmp"][63 * 128:64 * 128, :]), reads=[self.bscr], writes=[btd])
                    P.dma("sp", lambda e: e.dma_start(out=Sm["dbg_pg0"], in_=td[:]), reads=[btd], writes=[self.bout])
                    P.fence()
            self.smp_nsa(Sm)
            P.fence()
            block = st.enter_context(nc.Block())
            P.emit_all(block)
        return nc

    def smp_final(self, Sm, N, xs, xb, ov, wo1, wgf, wuf, wdf, o_ys):
        P = self.P
        with ExitStack() as st:
            mixT = self.sb(st, "mixTs1", [128, KC, 513], BF16)
            bmixT = Buf("mixTs1")
            tf = self.sb(st, "s_tf", [128, 16], F32)
            qmf = self.sb(st, "s_qmf", [128, 4], F32)
            qmT = self.sb(st, "s_qmb", [128, 4], BF16)
            mK = self.sb(st, "s_mK", [128, 4, 256], BF16)
            mV = self.sb(st, "s_mV", [128, 2, 512], BF16)
            mix = self.sb(st, "s_mix", [1, 512], F32)
            pp = self.sb(st, "s_pp", [1, 256], F32)
            pT = self.sb(st, "s_pT", [128, 2], BF16)
            mst = self.sb(st, "s_mst", [1, 8], F32)
            btf, bqm, bmem, bmix, bpp, bpT, bmst = [Buf() for _ in range(7)]
            P.dma("sp", lambda e: e.dma_start(out=tf[:, 0:12], in_=Sm["mixTs"]), reads=[self.bout], writes=[btf])
            for h in range(12):
                P.op("dve", lambda e, h=h: e.tensor_copy(out=mixT[:, h, 0:1], in_=tf[:, h:h + 1]), reads=[btf], writes=[bmixT])
            P.dma("sp", lambda e: e.dma_start(out=qmf[:], in_=N["qmTs_d"]), reads=[self.bnsa], writes=[bqm])
            P.op("dve", lambda e: e.tensor_copy(out=qmT[:, :], in_=qmf[:, :]), reads=[bqm], writes=[bqm])
            P.dma("pool", lambda e: e.dma_start(out=mK[:], in_=self.cmk[1]), writes=[bmem])
            P.dma("pool", lambda e: e.dma_start(out=mV[:], in_=self.cmv[1]), writes=[bmem])
            for h in range(4):
                pb = 4 + (h % 2) * 2
                P.op("pe", lambda e, h=h, pb=pb: e.matmul(self.ps[pb][0:1, 0:256], qmT[:, h:h + 1], mK[:, h, :], start=True, stop=True),
                     reads=[bqm, bmem], writes=[self.bps[pb]])
                P.op("dve", lambda e, pb=pb: e.reduce_max(out=mst[0:1, 0:1], in_=self.ps[pb][0:1, 0:256], axis=AX.X),
                     reads=[self.bps[pb]], writes=[bmst])
                P.op("dve", lambda e: e.tensor_scalar(mst[0:1, 0:1], mst[0:1, 0:1], -1.0, None, op0=ALU.mult), reads=[bmst], writes=[bmst])
                P.op("act", lambda e, pb=pb: e.activation(out=pp[0:1, :], in_=self.ps[pb][0:1, 0:256], func=AF.Exp, bias=mst[0:1, 0:1]),
                     reads=[self.bps[pb], bmst], writes=[bpp])
                P.op("dve", lambda e: e.reduce_sum(out=mst[0:1, 1:2], in_=pp[0:1, :], axis=AX.X), reads=[bpp, bmst], writes=[bmst])
                P.op("dve", lambda e: e.reciprocal(mst[0:1, 1:2], mst[0:1, 1:2]), reads=[bmst], writes=[bmst])
                for mt in range(2):
                    P.op("pe", lambda e, pb=pb, mt=mt: e.transpose(self.ps[pb + 1][0:128, mt:mt + 1], pp[0:1, mt * 128:(mt + 1) * 128],
                                                                   self.ident[0:1, 0:1]), reads=[bpp, self.bconst], writes=[self.bps[pb + 1]])
                P.op("act", lambda e, pb=pb: e.activation(out=pT[:, 0:2], in_=self.ps[pb + 1][0:128, 0:2], func=AF.Copy),
                     reads=[self.bps[pb + 1]], writes=[bpT])
                P.op("pe", [lambda e, pb=pb, mt=mt, h=h: e.matmul(self.ps[pb][0:1, 256:384], pT[:, mt:mt + 1], mV[:, mt, h * 128:(h + 1) * 128],
                                                                  start=(mt == 0), stop=(mt == 1)) for mt in range(2)],
                     reads=[bpT, bmem], writes=[self.bps[pb]])
                P.op("dve", lambda e, pb=pb, h=h: e.tensor_scalar(mix[0:1, h * 128:(h + 1) * 128], self.ps[pb][0:1, 256:384], mst[0:1, 1:2], None,
                                                                  op0=ALU.mult), reads=[self.bps[pb], bmst], writes=[bmix])
            for h in range(4):
                P.op("pe", lambda e, h=h: e.transpose(self.ps[0][0:128, h:h + 1], mix[0:1, h * 128:(h + 1) * 128], self.ident[0:1, 0:1]),
                     reads=[bmix, self.bconst], writes=[self.bps[0]])
            for h in range(4):
                P.op("act", lambda e, h=h: e.activation(out=mixT[:, 12 + h, 0:1], in_=self.ps[0][0:128, h:h + 1], func=AF.Copy),
                     reads=[self.bps[0]], writes=[bmixT])
            subt = [(0, 1)]
            P.dma("sp", lambda e: e.dma_start(out=xs[:, :, 0:1], in_=ov[:, :, 4 * SEG:4 * SEG + 1], allow_slow_non_contiguous=True), reads=[self.bout], writes=[xb])
            self.wout_pass(xs, xb, subt, wo1, mixT, bmixT, 6 + 3)
        self.ffn_pass(xs, xb, [(0, 1)], 1, 1, wgf, wuf, wdf)
        P.dma("sp", lambda e: e.dma_start(out=o_ys.rearrange("(c p) t -> p c t", p=128), in_=xs[:, :, 0:1], allow_slow_non_contiguous=True), reads=[xb], writes=[self.bout])
        P.fence()

    def build(self):
        nc = self.nc
        P = self.P
        stg = self.stages
        xall = self.din("xall", [D, 4 * SEG + 1])
        consts = self.din("consts", [128, 384])
        gall = self.din("gall", [128, 12 * KC])
        gon_d = self.din("gon", [128, 384])
        upto = stg.get("upto", 9)
        ffl = [(0, 0)] + ([(0, 1)] if upto >= 3 else []) + ([(1, 0)] if upto >= 4 else []) + ([(1, 1)] if upto >= 6 else [])
        wg = {k: self.din("wg%d%d" % k, [FT, 128, KC, 128]) for k in ffl}
        wu = {k: self.din("wu%d%d" % k, [FT, 128, KC, 128]) for k in ffl}
        wd = {k: self.din("wd%d%d" % k, [KC, 128, FT, 128]) for k in ffl}
        keep_d = self.din("keepv", [128, 8])
        if upto >= 4:
            wkvn = self.din("wkvn", [6, 128, KC, 384])
            swin = self.din("swin", [512, 768])
            o_kv = self.dout("o_kv", [4 * SEG + 1, 2304])
            o_win_s = self.dout("o_win_s", [512, 768])
        N = None
        self.bnsa = Buf("nsa")
        if upto >= 5:
            N = {"wnfm": self.din("wnfm", [28, 128, KC, 128]), "wgate": self.din("wgate", [128, KC, 36]),
                 "gate_b": self.din("gate_b", [1, 36]),
                 "kT_d": nc.dram_tensor("kT_d", [4, 3, 128, 4 * SEG], F32).ap(),
                 "qT_d": nc.dram_tensor("qT_d", [12, 128, SEG], F32).ap(),
                 "qmT_d": nc.dram_tensor("qmT_d", [4, 128, SEG], F32).ap(),
                 "gates_d": nc.dram_tensor("gates_d", [SEG, 36], F32).ap()}
            A = {"kT": N["kT_d"], "kvtok": o_kv, "qT": N["qT_d"], "gates": N["gates_d"],
                 "cmpbias": self.din("cmpbias", [8, 128, 768]), "selbias": self.din("selbias", [8, 128, 64]),
                 "nearT": self.din("nearT", [3, 5, 128, 512]), "cft": self.din("cft", [6, 3, 128]),
                 "rb31row": self.din("rb31row", [3, 3, 512]), "eexp": self.din("eexp", [64, 32, 128]),
                 "relb": self.din("relb", [1, 384]), "w1": self.din("w1", [2, 64, 128, 256]), "w2": self.din("w2", [2, 256, 128]),
                 "peT": self.din("peT", [2, 128, 64]), "b1c": self.din("b1c", [2, 128, 2]),
                 "tokmix": nc.dram_tensor("tokmix_d", [SEG, 1536], F32).ap()}
            N["tokmix"] = A["tokmix"]
            wo1 = self.din("wo1", [KC, 128, KC, 128])
            o_y = self.dout("o_y", [D, SEG])
            N["qTs_d"] = nc.dram_tensor("qTs_d", [128, 12], F32).ap()
            N["qmTs_d"] = nc.dram_tensor("qmTs_d", [128, 4], F32).ap()
            N["knew_d"] = nc.dram_tensor("knew_d", [128, 2, 3], F32).ap()
            N["gT_d"] = nc.dram_tensor("gT_d", [4, 9], F32).ap()
            N["wgT"] = self.din("wgT", [128, 9, KC, 4])
            N["gbT"] = self.din("gbT", [4, 9])
            Sm = {k: self.din(k, shp, dt) for k, shp, dt in [
                ("pool_cmp", [163840, 768], F32), ("pool_slc", [163840, 768], F32), ("ptrep", [128, 128], I32), ("iotap", [128, 1], F32),
                ("cfs", [2, 3, 128], F32), ("selb_s", [1, 256], F32), ("cmpb_s", [3, 4, 256], F32), ("eexp_s", [128, 64, 128], F32),
                ("nears_s", [3, 128, 4], F32), ("nearw_s", [3, 4, 128, 4], F32), ("b0_s", [1, 3, 4], F32), ("rb31_s", [3, 3, 4], F32)]}
            Sm.update({"swin": swin, "qTs_d": N["qTs_d"], "gT_d": N["gT_d"], "knew_d": N["knew_d"], "relb": A["relb"],
                       "w1": A["w1"], "w2": A["w2"], "peT": A["peT"], "b1c": A["b1c"],
                       "vnew_slc": o_kv[4 * SEG:4 * SEG + 1, 1152:1536], "vnew_win": o_kv[4 * SEG:4 * SEG + 1, 1920:2304],
                       "scr_cmp": nc.dram_tensor("scr_cmp", [16384, 768], F32).ap(),
                       "scr_slc": nc.dram_tensor("scr_slc", [16384, 768], F32).ap(),
                       "mixTs": nc.dram_tensor("mixTs_d", [128, 12], F32).ap()})
            o_ys = self.dout("o_ys", [D, 1])
        Wd = {"wq": self.din("wq", [8, 128, KC, 96]), "wk": self.din("wk", [8, 128, KC, 96]),
              "wqm": self.din("wqm", [4, 128, KC, 128]), "wa": self.din("wa", [128, KC, 16]),
              "wtm": self.din("wtm", [10, 128, KC, 384])}
        wo0 = self.din("wo0", [KC, 128, KC, 128])
        wa2 = self.din("wa2", [17, 768])
        self.sgla = self.din("sgla", [96, 8, 384])
        memT = self.din("memT", [D, 256])
        wmem = self.din("wmem", [2, 2, 128, KC, 512])
        wmemk = self.din("wmemk", [2, 4, 128, KC, 128])
        mgall = self.din("mgall", [128, 2 * KC])
        self.cmk = self.din("cmk", [2, 128, 4, 256])
        self.cmv = self.din("cmv", [2, 128, 2, 512])
        o_mem = self.dout("o_mem", [2, 256, 1024])
        self.o_gp = self.dout("o_gla_p", [96, 8, 384])
        self.o_gs = self.dout("o_gla_s", [96, 8, 384])
        o_x = self.dout("o_x", [D, 4 * SEG + 1])
        self.Sscr = nc.dram_tensor("Sscr", [96, 8, 384], F32).ap()
        self.memK_d = [nc.dram_tensor("memKd%d" % i, [128, 4, 256], F32).ap() for i in range(2)]
        self.memV_d = [nc.dram_tensor("memVd%d" % i, [128, 2, 512], F32).ap() for i in range(2)]
        self.bSscr, self.bmemd, self.bout = Buf("Sscr"), Buf("memd"), Buf("out")
        with ExitStack() as st:
            P.alloc_sems(st)
            self.ps = [st.enter_context(nc.psum_tensor("ps%d" % i, [128, 512], F32)) for i in range(8)]
            self.bps = [Buf("ps%d" % i) for i in range(8)]
            cst = self.sb(st, "cst", [128, 384], F32)
            self.ones = cst[:, 0:128]
            self.utri = cst[:, 128:256]
            self.ident = cst[:, 256:384]
            self.gsb = self.sb(st, "gsb", [128, 12 * KC], F32)
            self.wa2 = self.sb(st, "wa2s", [17, 768], F32)
            self.gon = self.sb(st, "gon", [128, 384], F32)
            self.bconst = Buf("const")
            self.rstd = self.sb(st, "rstd", [128, 520], F32)
            self.brstd = Buf("rstd")
            self.sq = [self.sb(st, "sq%d" % i, [128, 512], F32) for i in range(2)]
            self.bsq = [Buf() for _ in range(2)]
            mg = self.sb(st, "mg", [128, 2 * KC], F32)
            self.keepv = self.sb(st, "keepv", [128, 8], F32)
            P.dma("sp", lambda e: e.dma_start(out=self.keepv[:], in_=keep_d), writes=[self.bconst])
            P.dma("sp", lambda e: e.dma_start(out=cst[:], in_=consts), writes=[self.bconst])
            P.dma("sp", lambda e: e.dma_start(out=self.gsb[:], in_=gall), writes=[self.bconst])
            P.dma("sp", lambda e: e.dma_start(out=self.wa2[:], in_=wa2), writes=[self.bconst])
            P.dma("sp", lambda e: e.dma_start(out=self.gon[:], in_=gon_d), writes=[self.bconst])
            P.dma("sp", lambda e: e.dma_start(out=mg[:], in_=mgall), writes=[self.bconst])
            with ExitStack() as stz:
                zt = self.sb(stz, "zt", [96, 8, 384], F32)
                bz = Buf()
                P.op("dve", lambda e: e.memset(zt[:, :, :], 0.0), writes=[bz])
                P.dma("sp", lambda e: e.dma_start(out=self.Sscr, in_=zt[:]), reads=[bz], writes=[self.bSscr])
                P.fence()
            self.mem_kv_phase(memT, wmem, mg, o_mem, wmemk)
            self.sstg = [self.sb(st, "sstg%d" % i, [128, 4], F32) for i in range(2)]
            self.bsstg = [Buf(), Buf()]
            if upto >= 5:
                self.smp_pregather(Sm)
            xs = self.sb(st, "xs", [128, KC, 513], F32)
            xb = Buf("x")
            xv = xall.rearrange("(c p) t -> p c t", p=128)
            ov = o_x.rearrange("(c p) t -> p c t", p=128)
            npass = stg.get("npass", 8)
            for pi in range(8 - npass, 8):
                t0 = pi * 512
                last = (pi == 7)
                if last:
                    P.dma("sp", lambda e, t0=t0: e.dma_start(out=xs[:, :, 0:513], in_=xv[:, :, t0:t0 + 513]), writes=[xb])
                    subt = [(0, 512), (512, 1)]
                else:
                    P.dma("sp", lambda e, t0=t0: e.dma_start(out=xs[:, :, 0:512], in_=xv[:, :, t0:t0 + 512]), writes=[xb])
                    subt = [(0, 512)]
                Wt = 513 if last else 512
                self.ffn_pass(xs, xb, subt, 0, 0, wg[(0, 0)], wu[(0, 0)], wd[(0, 0)])
                if upto >= 2:
                    with ExitStack() as stm:
                        mixT = self.sb(stm, "mixT", [128, KC, 513], BF16)
                        bmixT = Buf("mixT")
                        self.gla_pass(xs, xb, subt, Wd, mixT, bmixT, last, pi)
                        self.wout_pass(xs, xb, subt, wo0, mixT, bmixT, 3)
                if upto >= 3:
                    self.ffn_pass(xs, xb, subt, 0, 1, wg[(0, 1)], wu[(0, 1)], wd[(0, 1)])
                if upto >= 4:
                    self.ffn_pass(xs, xb, subt, 1, 0, wg[(1, 0)], wu[(1, 0)], wd[(1, 0)])
                    own_off = (pi - 6) * 512 if (N is not None and pi >= 6) else None
                    self.nsa_kv_pass(xs, xb, subt, wkvn, o_kv, t0, swin, o_win_s, N, own_off)
                P.dma("sp", lambda e, t0=t0, Wt=Wt: e.dma_start(out=ov[:, :, t0:t0 + Wt], in_=xs[:, :, 0:Wt]),
                      reads=[xb], writes=[self.bout])
            P.fence()
            if upto >= 5:
                with ExitStack() as stn:
                    kcmpT, vcmp, bkc = self.nsa_compress(stn, A)
                    self.nsa_attn(A, list(range(8)), kcmpT, vcmp, bkc)
                oy = o_y.rearrange("(c p) t -> p c t", p=128)
                for hf in range(2):
                    off = hf * 512
                    subt = [(0, 512)]
                    P.dma("sp", lambda e, off=off: e.dma_start(out=xs[:, :, 0:512], in_=ov[:, :, 3 * SEG + off:3 * SEG + off + 512]),
                          reads=[self.bout], writes=[xb])
                    with ExitStack() as stm:
                        mixT = self.sb(stm, "mixT1", [128, KC, 513], BF16)
                        bmixT = Buf("mixT1")
                        self.nsa_mix_pass(N, off, mixT, bmixT)
                        self.wout_pass(xs, xb, subt, wo1, mixT, bmixT, 6 + 3)
                    if upto >= 6:
                        self.ffn_pass(xs, xb, subt, 1, 1, wg[(1, 1)], wu[(1, 1)], wd[(1, 1)])
                    P.dma("sp", lambda e, off=off: e.dma_start(out=oy[:, :, off:off + 512], in_=xs[:, :, 0:512]),
                          reads=[xb], writes=[self.bout])
                P.fence()
                self.smp_nsa(Sm)
                self.smp_final(Sm, N, xs, xb, ov, wo1, wg[(1, 1)], wu[(1, 1)], wd[(1, 1)], o_ys)
            block = st.enter_context(nc.Block())
            P.emit_all(block)
        return nc


def _slab_kc(w, ncol):
    C = w.shape[1]
    return np.ascontiguousarray(w.reshape(KC, 128, C // ncol, ncol).transpose(2, 1, 0, 3))


def _slab_down(w):
    return np.ascontiguousarray(w.reshape(FT, 128, KC, 128).transpose(2, 1, 0, 3))


def _consts():
    c = np.zeros((128, 384), np.float32)
    c[:, 0:128] = 1.0
    c[:, 128:256] = np.triu(np.ones((128, 128), np.float32))
    c[:, 256:384] = np.eye(128, dtype=np.float32)
    return c


def seg_order(j):
    return [(j + 1 + r) % 4 for r in range(4)]


def make_in_maps(inp, stages):
    x_prompt = inp["x_prompt"]
    gall = np.ascontiguousarray(inp["norm_g"].reshape(12, KC, 128).transpose(2, 0, 1).reshape(128, 12 * KC))
    shared = {"consts": _consts(), "gall": gall}
    upto = stages.get("upto", 9)
    ffl = [(0, 0)] + ([(0, 1)] if upto >= 3 else []) + ([(1, 0)] if upto >= 4 else []) + ([(1, 1)] if upto >= 6 else [])
    if upto >= 4:
        shared["wkvn"] = _slab_kc(inp["w_in_nsa"][0][:, 1536:3840], 384)
    if upto >= 5:
        wn = inp["w_in_nsa"][0]
        kcols = np.concatenate([wn[:, 1536 + t * 384:1536 + (t + 1) * 384] for t in (0, 1, 2, 4)], axis=1)
        shared["wnfm"] = _slab_kc(np.concatenate([kcols, wn[:, 0:1536], wn[:, 3876:4388]], axis=1), 128)
        shared["wgate"] = _slab_kc(wn[:, 3840:3876], 36)[0]
        shared["gate_b"] = inp["nsa_gate_b"][0].reshape(1, 36).astype(np.float32)
        shared["relb"] = inp["rel_bias"].reshape(1, 384).astype(np.float32)
        shared["w1"] = np.ascontiguousarray(inp["cmp_w1"][0].reshape(2, 64, 128, 256))
        shared["w2"] = np.ascontiguousarray(inp["cmp_w2"][0])
        shared["peT"] = np.ascontiguousarray(inp["cmp_pe"][0].transpose(0, 2, 1))
        shared["b1c"] = np.ascontiguousarray(inp["cmp_b1"][0].reshape(2, 2, 128).transpose(0, 2, 1))
        shared["wo1"] = np.ascontiguousarray(inp["w_out"][1].reshape(KC, 128, KC, 128).transpose(2, 1, 0, 3))
        gcols = np.array([[3840 + 3 * (4 * g + h) + br for h in range(4)] for br in range(3) for g in range(3)])
        wg9 = wn[:, gcols.reshape(-1)].reshape(KC, 128, 9, 4)
        shared["wgT"] = np.ascontiguousarray(wg9.transpose(1, 2, 0, 3))
        shared["gbT"] = np.ascontiguousarray(inp["nsa_gate_b"][0][gcols - 3840].T).astype(np.float32)
        shared.update(smp_tables(inp["rel_bias"]))
        shared["pool_cmp"] = inp["cache_cmp_kv"][0].reshape(163840, 768)
        shared["pool_slc"] = inp["cache_slc_kv"][0].reshape(163840, 768)
    for (i, j) in ffl:
        shared["wg%d%d" % (i, j)] = _slab_kc(inp["w_ffn_gate"][i, j], 128)
        shared["wu%d%d" % (i, j)] = _slab_kc(inp["w_ffn_up"][i, j], 128)
        shared["wd%d%d" % (i, j)] = _slab_down(inp["w_ffn_down"][i, j])
    wgla = inp["w_in_gla"][0]
    shared["wq"] = _slab_kc(wgla[:, 0:768], 96)
    shared["wk"] = _slab_kc(wgla[:, 768:1536], 96)
    shared["wqm"] = _slab_kc(wgla[:, 4624:5136], 128)
    shared["wa"] = _slab_kc(wgla[:, 4608:4624], 16)[0]
    shared["wtm"] = _slab_kc(wgla[:, 768:4608], 384)
    shared["wo0"] = np.ascontiguousarray(inp["w_out"][0].reshape(KC, 128, KC, 128).transpose(2, 1, 0, 3))
    shared["wa2"] = np.concatenate([inp["w_gla_a2"][0], inp["b_gla_a"][0][None, :]], axis=0).astype(np.float32)
    shared["gon"] = np.ascontiguousarray(np.broadcast_to(inp["gla_onorm_g"][0][None, :], (128, 384))).astype(np.float32)
    shared["wmem"] = np.stack([_slab_kc(inp["w_mem_kv"][i], 512) for i in range(2)])
    shared["wmemk"] = np.stack([_slab_kc(inp["w_mem_kv"][i][:, 0:512], 128) for i in range(2)])
    shared["mgall"] = np.ascontiguousarray(inp["mem_norm_g"].reshape(2, KC, 128).transpose(2, 0, 1).reshape(128, 2 * KC))
    maps = []
    for c in range(8):
        b, j = c // 4, c % 4
        order = seg_order(j)
        xall = np.empty((D, 4 * SEG + 1), np.float32)
        for r, sg_ in enumerate(order):
            xall[:, r * SEG:(r + 1) * SEG] = x_prompt[b, sg_ * SEG:(sg_ + 1) * SEG, :].T
        xall[:, 4 * SEG] = inp["x_sample"][c, 0, :]
        keepv = np.ones((128, 8), np.float32)
        if j < 3:
            keepv[:, 2 * (3 - j)] = 0.0
        sg = inp["state_gla"][0, c]
        sgla = np.ascontiguousarray(sg.reshape(8, 96, 384).transpose(1, 0, 2))
        cm = inp["cache_mem_kv"][:, c]
        cmk = np.ascontiguousarray(cm[:, :, 0].transpose(0, 3, 2, 1))
        cmv = np.ascontiguousarray(cm[:, :, 1].reshape(2, 2, 128, 512).transpose(0, 2, 1, 3))
        m = dict(shared)
        if upto >= 5:
            m.update(nsa_tables(inp["rel_bias"], j))
            m["ptrep"] = np.ascontiguousarray(np.broadcast_to(inp["page_table"][c][None, :], (128, 128))).astype(np.int32)
        if upto >= 4:
            m["swin"] = np.ascontiguousarray(inp["state_win_kv"][0, c].reshape(512, 768))
        m.update({"xall": xall, "sgla": sgla, "cmk": cmk, "cmv": cmv, "keepv": keepv,
                  "memT": np.ascontiguousarray(inp["mem_prompt"][b].T)})
        maps.append(m)
    return maps


_CACHE = {}


def run_device(inp, stages):
    key = tuple(sorted(stages.items()))
    if key not in _CACHE:
        _CACHE[key] = Builder(stages).build()
    nc = _CACHE[key]
    maps = make_in_maps(inp, stages)
    res = run_bass_kernel_spmd(nc, maps, core_ids=list(range(8)))
    return res.results


def kernel(**inp):
    inp = {k: np.asarray(v) for k, v in inp.items()}
    r = run_device(inp, {"upto": 6})
    y_prompt = np.zeros((2, 4096, 2048), np.float32)
    y_sample = np.zeros((8, 1, 2048), np.float32)
    gla_p = np.zeros((1, 2, 4, 192, 384), np.float32)
    gla_s = np.zeros((1, 8, 4, 192, 384), np.float32)
    cmp_p = np.zeros((1, 2, 4096, 2, 3, 128), np.float32)
    slc_p = np.zeros((1, 2, 4096, 2, 3, 128), np.float32)
    win_p = np.zeros((1, 2, 512, 2, 3, 128), np.float32)
    mem_p = np.zeros((2, 2, 256, 2, 4, 128), np.float32)
    cmp_s = np.zeros((1, 8, 1, 2, 3, 128), np.float32)
    slc_s = np.zeros((1, 8, 1, 2, 3, 128), np.float32)
    win_s = np.zeros((1, 8, 512, 2, 3, 128), np.float32)
    for c in range(8):
        b, j = c // 4, c % 4
        rc = r[c]
        own = slice(3 * SEG, 4 * SEG)
        y_prompt[b, j * SEG:(j + 1) * SEG] = rc["o_y"].T
        y_sample[c, 0] = rc["o_ys"][:, 0]
        gla_s[0, c] = rc["o_gla_s"].transpose(1, 0, 2).reshape(4, 192, 384)
        kv = rc["o_kv"]
        cmp_p[0, b, j * SEG:(j + 1) * SEG] = kv[own, 0:768].reshape(SEG, 2, 3, 128)
        slc_p[0, b, j * SEG:(j + 1) * SEG] = kv[own, 768:1536].reshape(SEG, 2, 3, 128)
        cmp_s[0, c, 0] = kv[4 * SEG, 0:768].reshape(2, 3, 128)
        slc_s[0, c, 0] = kv[4 * SEG, 768:1536].reshape(2, 3, 128)
        win_s[0, c] = rc["o_win_s"].reshape(512, 2, 3, 128)
        if j == 3:
            gla_p[0, b] = rc["o_gla_p"].transpose(1, 0, 2).reshape(4, 192, 384)
            win_p[0, b] = kv[4 * SEG - 512:4 * SEG, 1536:2304].reshape(512, 2, 3, 128)
        if j == 0:
            mem_p[:, b] = rc["o_mem"].reshape(2, 256, 2, 4, 128)
    cmp_p = cmp_p.reshape(1, 2, 32, 128, 2, 3, 128)
    slc_p = slc_p.reshape(1, 2, 32, 128, 2, 3, 128)
    return (y_prompt, y_sample, gla_p, cmp_p, slc_p, win_p, mem_p, gla_s, cmp_s, slc_s, win_s)


NEG = -30000.0


def t5_bucket_np(dist):
    n = np.maximum(dist, 0)
    nf = np.maximum(n, 16).astype(np.float32)
    large = 16 + (np.log(nf / np.float32(16.0)) / np.float32(np.log(128.0 / 16.0)) * np.float32(16.0)).astype(np.int32)
    return np.where(n < 16, n, np.minimum(large, 31)).astype(np.int64)


def nsa_tables(rel_bias, j):
    order = seg_order(j)
    q = np.arange(128)
    cmpbias = np.empty((8, 128, 12, 64), np.float32)
    selbias = np.zeros((8, 128, 64), np.float32)
    nb = np.arange(64)
    n_real = np.array([order[b // 16] * 16 + b % 16 for b in nb])
    for qi in range(8):
        t = j * 1024 + qi * 128 + q
        dist = t[:, None] - (64 * n_real[None, :] + 63)
        bias = rel_bias[t5_bucket_np(dist)]
        bias = np.where((dist >= 0)[:, :, None], bias, np.float32(NEG))
        cmpbias[qi] = bias.transpose(0, 2, 1)
        tb = t // 64
        forced = (n_real[None, :] == 0) | (n_real[None, :] == tb[:, None]) | (n_real[None, :] == tb[:, None] - 1)
        future = n_real[None, :] * 64 > t[:, None]
        selbias[qi] = np.where(forced, 1e4, np.where(future, -1e4, 0.0)).astype(np.float32)
    nearT = np.empty((3, 5, 128, 4, 128), np.float32)
    for kk in range(5):
        dist = kk * 128 + q[None, :] - q[:, None]
        ok = (dist >= 0) & (dist <= 512)
        b = rel_bias[t5_bucket_np(dist)]
        b = np.where(ok[:, :, None], b, np.float32(NEG))
        for g in range(3):
            nearT[g, kk] = b[:, :, 4 * g:4 * g + 4].transpose(0, 2, 1)
    valid = [order[r] < j for r in range(3)]
    cft = np.zeros((6, 3, 128), np.float32)
    cft[:, 0, :] = 1.0
    for r in range(3):
        if valid[r]:
            cft[r, 1, :] = 1.0
        else:
            cft[r, 2, :] = NEG
    if not valid[2]:
        cft[3, 2, :] = NEG
    cft[4, 1, :] = 1.0
    rb31row = np.empty((3, 3, 512), np.float32)
    for g in range(3):
        rb31row[g, 0] = 0.0
        rb31row[g, 1] = np.repeat(rel_bias[31, 4 * g:4 * g + 4], 128)
        rb31row[g, 2] = 1.0
    eexp = np.zeros((64, 32, 128), np.float32)
    for kt in range(32):
        eexp[2 * kt, kt, 0:64] = 1.0
        eexp[2 * kt + 1, kt, 64:128] = 1.0
    return {"cmpbias": cmpbias.reshape(8, 128, 768), "selbias": selbias, "nearT": nearT.reshape(3, 5, 128, 512),
            "cft": cft, "rb31row": rb31row, "eexp": eexp}


def smp_tables(rel_bias):
    n = np.arange(256)
    cmpb = rel_bias[t5_bucket_np(16384 - (64 * n + 63))]
    cmpb_s = np.ascontiguousarray(cmpb.T.reshape(3, 4, 256)).astype(np.float32)
    selb_s = np.zeros((1, 256), np.float32)
    selb_s[0, 0] = 1e4
    selb_s[0, 255] = 1e4
    k = np.arange(128)
    ns = rel_bias[t5_bucket_np(128 - k)]
    nears_s = np.ascontiguousarray(ns.reshape(128, 3, 4).transpose(1, 0, 2)).astype(np.float32)
    i = np.arange(512)
    nw = rel_bias[t5_bucket_np(512 - i)]
    nearw_s = np.ascontiguousarray(nw.reshape(4, 128, 3, 4).transpose(2, 0, 1, 3)).astype(np.float32)
    b0_s = rel_bias[0].reshape(1, 3, 4).astype(np.float32)
    rb31_s = np.zeros((3, 3, 4), np.float32)
    rb31_s[:, 1, :] = rel_bias[31].reshape(3, 4)
    rb31_s[:, 2, :] = 1.0
    cfs = np.zeros((2, 3, 128), np.float32)
    cfs[:, 0, :] = 1.0
    cfs[0, 1, :] = 1.0
    eexp_s = np.zeros((128, 64, 128), np.float32)
    for pi_ in range(64):
        eexp_s[2 * pi_, pi_, 0:64] = 1.0
        eexp_s[2 * pi_ + 1, pi_, 64:128] = 1.0
    return {"cmpb_s": cmpb_s, "selb_s": selb_s, "nears_s": nears_s, "nearw_s": nearw_s, "b0_s": b0_s, "rb31_s": rb31_s,
            "cfs": cfs, "eexp_s": eexp_s, "iotap": np.arange(128, dtype=np.float32).reshape(128, 1)}


def _build_pg_test(self):
    nc = self.nc
    P = self.P
    Sm = {k: self.din(k, shp, dt) for k, shp, dt in [
        ("pool_cmp", [163840, 768], F32), ("pool_slc", [163840, 768], F32), ("ptrep", [128, 128], I32), ("iotap", [128, 1], F32)]}
    Sm["scr_cmp"] = nc.dram_tensor("scr_cmp", [16384, 768], F32).ap()
    Sm["scr_slc"] = nc.dram_tensor("scr_slc", [16384, 768], F32).ap()
    o1 = self.dout("o1", [128, 768])
    o2 = self.dout("o2", [128, 768])
    self.bout, self.bnsa = Buf("out"), Buf("nsa")
    with ExitStack() as st:
        P.alloc_sems(st)
        self.smp_pregather(Sm)
        t1 = self.sb(st, "t1", [128, 768], F32)
        t2 = self.sb(st, "t2", [128, 768], F32)
        b1, b2 = Buf(), Buf()
        P.dma("sp", lambda e: e.dma_start(out=t1[:], in_=Sm["scr_cmp"][0:128, :]), reads=[self.bscr], writes=[b1])
        P.dma("sp", lambda e: e.dma_start(out=t2[:], in_=Sm["scr_slc"][127 * 128:128 * 128, :]), reads=[self.bscr], writes=[b2])
        P.dma("sp", lambda e: e.dma_start(out=o1, in_=t1[:]), reads=[b1], writes=[self.bout])
        P.dma("sp", lambda e: e.dma_start(out=o2, in_=t2[:]), reads=[b2], writes=[self.bout])
        P.fence()
        block = st.enter_context(nc.Block())
        P.emit_all(block)
    return nc


Builder.build_pg_test = _build_pg_test
```
